# Optimizing a Trainium2 kernel written in Bass

```python
import math
import jax, jax.numpy as jnp
from jax import lax
import numpy as np

D_MODEL = 1024
BATCH = 8
SEQ = 4096
DEPTH = 2

N_MIXERS = 2
CHUNK = 64
NORM_EPS = 1e-6
L2_EPS = 1e-6

GLA_HEADS = 4
GLA_KEY = D_MODEL // 2
GLA_VAL = D_MODEL
GLA_DK = GLA_KEY // GLA_HEADS
GLA_DV = GLA_VAL // GLA_HEADS
GLA_RANK = 16
GLA_TAU = 16.0
GLA_IN = 2 * GLA_KEY + 2 * GLA_VAL + GLA_RANK

GDN_HEAD_DIM = 128
GDN_QK_HEADS = D_MODEL // GDN_HEAD_DIM
GDN_V_HEADS = 2 * GDN_QK_HEADS
GDN_KEY = GDN_QK_HEADS * GDN_HEAD_DIM
GDN_VAL = GDN_V_HEADS * GDN_HEAD_DIM
GDN_CONV = 4
GDN_CONV_CH = 2 * GDN_KEY + GDN_VAL
GDN_IN = GDN_CONV_CH + GDN_VAL + 2 * GDN_V_HEADS

D_FF = -(-8 * D_MODEL // (3 * 256)) * 256

kernel_name = 'gla_gated_deltanet_interleaved_hybrid'


def rms_norm(x, w):
    xf = x.astype(jnp.float32)
    y = xf * lax.rsqrt(jnp.mean(xf * xf, axis=-1, keepdims=True) + NORM_EPS)
    return (y * w.astype(jnp.float32)).astype(x.dtype)


def l2_norm(x):
    xf = x.astype(jnp.float32)
    return xf * lax.rsqrt(jnp.sum(xf * xf, axis=-1, keepdims=True) + L2_EPS)


def to_chunks(t):
    b, l, h, d = t.shape
    return t.reshape(b, l // CHUNK, CHUNK, h, d).transpose(1, 0, 3, 2, 4)


def from_chunks(t):
    n, b, h, c, d = t.shape
    return t.transpose(1, 0, 3, 2, 4).reshape(b, n * c, h, d)


def causal_depthwise_conv(x, w):
    k = w.shape[0]
    return lax.conv_general_dilated(
        x, w[:, None, :], window_strides=(1,), padding=[(k - 1, 0)],
        dimension_numbers=('NWC', 'WIO', 'NWC'), feature_group_count=x.shape[-1])


def chunked_gla(q, k, v, log_a):
    causal = jnp.tril(jnp.ones((CHUNK, CHUNK), bool))[:, :, None]
    _, b, h, _, dk = q.shape
    dv = v.shape[-1]

    def step(S, inp):
        qc, kc, vc, gc = inp
        G = jnp.cumsum(gc, axis=-2)
        diff = G[..., :, None, :] - G[..., None, :, :]
        decay = jnp.exp(jnp.where(causal, diff, -jnp.inf))
        scores = jnp.einsum('bhid,bhjd,bhijd->bhij', qc, kc, decay)
        o = scores @ vc + jnp.einsum('bhid,bhde->bhie', qc * jnp.exp(G), S)
        G_last = G[..., -1:, :]
        k_dec = kc * jnp.exp(G_last - G)
        S = jnp.exp(G_last)[..., 0, :, None] * S + jnp.einsum('bhjd,bhje->bhde', k_dec, vc)
        return S, o

    S0 = jnp.zeros((b, h, dk, dv), jnp.float32)
    _, o = lax.scan(step, S0, (q, k, v, log_a))
    return o


def chunked_gated_delta(q, k, v, g, beta):
    causal = jnp.tril(jnp.ones((CHUNK, CHUNK), bool))
    strict = jnp.tril(jnp.ones((CHUNK, CHUNK), bool), -1)
    eye = jnp.eye(CHUNK, dtype=jnp.float32)
    _, b, h, _, dk = q.shape
    dv = v.shape[-1]

    def step(S, inp):
        qc, kc, vc, gc, bc = inp
        G = jnp.cumsum(gc, axis=-1)
        decay = jnp.exp(jnp.where(causal, G[..., :, None] - G[..., None, :], -jnp.inf))
        kb = kc * bc[..., None]
        A = jnp.where(strict, jnp.einsum('bhid,bhjd->bhij', kb, kc) * decay, 0.0)
        T = lax.linalg.triangular_solve(eye + A, jnp.broadcast_to(eye, A.shape),
                                        left_side=True, lower=True)
        u = T @ (vc * bc[..., None])
        w = T @ (kb * jnp.exp(G)[..., None])
        v_new = u - w @ S
        attn = jnp.where(causal, jnp.einsum('bhid,bhjd->bhij', qc, kc) * decay, 0.0)
        o = jnp.einsum('bhid,bhde->bhie', qc * jnp.exp(G)[..., None], S) + attn @ v_new
        G_last = G[..., -1]
        k_dec = kc * jnp.exp(G_last[..., None] - G)[..., None]
        S = jnp.exp(G_last)[..., None, None] * S + jnp.einsum('bhjd,bhje->bhde', k_dec, v_new)
        return S, o

    S0 = jnp.zeros((b, h, dk, dv), jnp.float32)
    _, o = lax.scan(step, S0, (q, k, v, g, beta))
    return o


def gla_mixer(h, w_in, w_gate_up, b_gate, norm_w, w_out):
    bsz, l, _ = h.shape
    proj = h @ w_in
    q, k, v, r, g_low = jnp.split(
        proj, [GLA_KEY, 2 * GLA_KEY, 2 * GLA_KEY + GLA_VAL, 2 * GLA_KEY + 2 * GLA_VAL], axis=-1)
    log_a = jax.nn.log_sigmoid((g_low @ w_gate_up + b_gate).astype(jnp.float32)) / GLA_TAU
    qh = to_chunks(q.astype(jnp.float32).reshape(bsz, l, GLA_HEADS, GLA_DK) * GLA_DK ** -0.5)
    kh = to_chunks(k.astype(jnp.float32).reshape(bsz, l, GLA_HEADS, GLA_DK))
    vh = to_chunks(v.astype(jnp.float32).reshape(bsz, l, GLA_HEADS, GLA_DV))
    gh = to_chunks(log_a.reshape(bsz, l, GLA_HEADS, GLA_DK))
    o = from_chunks(chunked_gla(qh, kh, vh, gh))
    o = rms_norm(o, norm_w) * jax.nn.silu(r.astype(jnp.float32).reshape(bsz, l, GLA_HEADS, GLA_DV))
    return o.reshape(bsz, l, GLA_VAL).astype(h.dtype) @ w_out


def gdn_mixer(h, w_in, conv_w, a_log, dt_bias, norm_w, w_out):
    bsz, l, _ = h.shape
    proj = h @ w_in
    qkv, z, b, a = jnp.split(
        proj, [GDN_CONV_CH, GDN_CONV_CH + GDN_VAL, GDN_CONV_CH + GDN_VAL + GDN_V_HEADS], axis=-1)
    qkv = jax.nn.silu(causal_depthwise_conv(qkv, conv_w))
    q, k, v = jnp.split(qkv, [GDN_KEY, 2 * GDN_KEY], axis=-1)
    rep = GDN_V_HEADS // GDN_QK_HEADS
    q = jnp.repeat(l2_norm(q.reshape(bsz, l, GDN_QK_HEADS, GDN_HEAD_DIM)), rep, axis=2)
    k = jnp.repeat(l2_norm(k.reshape(bsz, l, GDN_QK_HEADS, GDN_HEAD_DIM)), rep, axis=2)
    v = v.astype(jnp.float32).reshape(bsz, l, GDN_V_HEADS, GDN_HEAD_DIM)
    beta = jax.nn.sigmoid(b.astype(jnp.float32))
    g = -jnp.exp(a_log.astype(jnp.float32)) * jax.nn.softplus(
        a.astype(jnp.float32) + dt_bias.astype(jnp.float32))
    o = chunked_gated_delta(to_chunks(q * GDN_HEAD_DIM ** -0.5), to_chunks(k), to_chunks(v),
                            to_chunks(g[..., None])[..., 0], to_chunks(beta[..., None])[..., 0])
    o = from_chunks(o)
    o = rms_norm(o, norm_w) * jax.nn.silu(z.astype(jnp.float32).reshape(bsz, l, GDN_V_HEADS, GDN_HEAD_DIM))
    return o.reshape(bsz, l, GDN_VAL).astype(h.dtype) @ w_out


def swiglu(h, w_gate_up, w_down):
    gate, up = jnp.split(h @ w_gate_up, 2, axis=-1)
    return (jax.nn.silu(gate) * up) @ w_down


def setup_inputs(seed: int = 0) -> dict:
    key = jax.random.key(seed)
    ks = jax.random.split(key, 20)
    f32 = jnp.float32
    n_gla = (DEPTH + 1) // 2
    n_gdn = DEPTH // 2

    def dense(k, shape, fan_in):
        return jax.random.normal(k, shape, f32) * fan_in ** -0.5

    def gain(k, shape):
        return 1.0 + 0.01 * jax.random.normal(k, shape, f32)

    x = jax.random.normal(ks[0], (BATCH, SEQ, D_MODEL), f32)
    gla_w_in = dense(ks[1], (n_gla, D_MODEL, GLA_IN), D_MODEL)
    gla_w_gate_up = dense(ks[2], (n_gla, GLA_RANK, GLA_KEY), GLA_RANK)
    gla_b_gate = 0.1 * jax.random.normal(ks[3], (n_gla, GLA_KEY), f32)
    gla_norm_w = gain(ks[4], (n_gla, GLA_DV))
    gla_w_out = dense(ks[5], (n_gla, GLA_VAL, D_MODEL), GLA_VAL)
    gdn_w_in = dense(ks[6], (n_gdn, D_MODEL, GDN_IN), D_MODEL)
    gdn_conv_w = dense(ks[7], (n_gdn, GDN_CONV, GDN_CONV_CH), GDN_CONV)
    gdn_a_log = jnp.log(jax.random.uniform(ks[8], (n_gdn, GDN_V_HEADS), f32, 1.0, 16.0))
    dt = jnp.exp(jax.random.uniform(ks[9], (n_gdn, GDN_V_HEADS), f32,
                                    math.log(1e-3), math.log(1e-1)))
    gdn_dt_bias = dt + jnp.log(-jnp.expm1(-dt))
    gdn_norm_w = gain(ks[10], (n_gdn, GDN_HEAD_DIM))
    gdn_w_out = dense(ks[11], (n_gdn, GDN_VAL, D_MODEL), GDN_VAL)
    mix_norm_w = gain(ks[12], (DEPTH, D_MODEL))
    ffn_norm_w = gain(ks[13], (DEPTH, D_MODEL))
    ffn_w_gate_up = dense(ks[14], (DEPTH, D_MODEL, 2 * D_FF), D_MODEL)
    ffn_w_down = dense(ks[15], (DEPTH, D_FF, D_MODEL), D_FF)
    final_norm_w = gain(ks[16], (D_MODEL,))
    return {
        'x': x,
        'gla_w_in': gla_w_in, 'gla_w_gate_up': gla_w_gate_up, 'gla_b_gate': gla_b_gate,
        'gla_norm_w': gla_norm_w, 'gla_w_out': gla_w_out,
        'gdn_w_in': gdn_w_in, 'gdn_conv_w': gdn_conv_w, 'gdn_a_log': gdn_a_log,
        'gdn_dt_bias': gdn_dt_bias, 'gdn_norm_w': gdn_norm_w, 'gdn_w_out': gdn_w_out,
        'mix_norm_w': mix_norm_w, 'ffn_norm_w': ffn_norm_w,
        'ffn_w_gate_up': ffn_w_gate_up, 'ffn_w_down': ffn_w_down,
        'final_norm_w': final_norm_w,
    }


def reference(x, gla_w_in, gla_w_gate_up, gla_b_gate, gla_norm_w, gla_w_out,
              gdn_w_in, gdn_conv_w, gdn_a_log, gdn_dt_bias, gdn_norm_w, gdn_w_out,
              mix_norm_w, ffn_norm_w, ffn_w_gate_up, ffn_w_down, final_norm_w):
    h = x
    for i in range(DEPTH):
        j = i // N_MIXERS
        hn = rms_norm(h, mix_norm_w[i])
        if i % N_MIXERS == 0:
            h = h + gla_mixer(hn, gla_w_in[j], gla_w_gate_up[j], gla_b_gate[j],
                              gla_norm_w[j], gla_w_out[j])
        else:
            h = h + gdn_mixer(hn, gdn_w_in[j], gdn_conv_w[j], gdn_a_log[j],
                              gdn_dt_bias[j], gdn_norm_w[j], gdn_w_out[j])
        h = h + swiglu(rms_norm(h, ffn_norm_w[i]), ffn_w_gate_up[i], ffn_w_down[i])
    return rms_norm(h, final_norm_w)
```

```python
import numpy as np
from contextlib import ExitStack
import concourse.bass as bass
import concourse.mybir as mybir
from concourse.bass_utils import run_bass_kernel_spmd

F32 = mybir.dt.float32
BF16 = mybir.dt.bfloat16
AF = mybir.ActivationFunctionType
ALU = mybir.AluOpType
AX = mybir.AxisListType

D = 1024
SEQ = 4096
T = 512
NCH = T // 128
DFF = 2816
NFC = DFF // 128
BLK = 4096
NORM_EPS = 1e-6
L2_EPS = 1e-6
NEUMANN_BF16 = True
NSETS = 3
B_GAP = 6
import os
DBG = {k: True for k in os.environ.get('KDBG', '').split(',') if k}

BLOCKS = (["gla_q", "gla_k", "gla_v0", "gla_v1", "gla_r0", "gla_r1", "gla_o0", "gla_o1"]
          + ["f0_gu%d" % i for i in range(11)] + ["f0_d%d" % i for i in range(8)]
          + ["gdn_qkv%d" % i for i in range(8)] + ["gdn_z%d" % i for i in range(4)]
          + ["gdn_o%d" % i for i in range(4)]
          + ["f1_gu%d" % i for i in range(11)] + ["f1_d%d" % i for i in range(8)])
NBLK = len(BLOCKS)
BIDX = {n: i for i, n in enumerate(BLOCKS)}

C_IDENT, C_MASKT, C_TRIS, C_LSTRICT, C_MASKD, C_MASKOFF, C_ONES, C_SEL = 0, 128, 256, 384, 512, 640, 768, 896
NCST = 896 + 2048
C_SELB = 896


def _pack_kmajor(W):
    nc_ = W.shape[1] // 128
    return W.reshape(8, 128, nc_, 128).transpose(1, 2, 0, 3)


def make_wblocks(inp):
    wb = np.zeros((NBLK, 128, BLK), np.float32)

    def put(name, arr):
        a = np.ascontiguousarray(arr).reshape(128, -1)
        wb[BIDX[name], :, :a.shape[1]] = a

    gw = inp["gla_w_in"][0]
    put("gla_q", _pack_kmajor(gw[:, 0:512]))
    put("gla_k", _pack_kmajor(gw[:, 512:1024]))
    for cg in range(2):
        put("gla_v%d" % cg, gw[:, 1024 + cg * 512:1024 + (cg + 1) * 512].reshape(8, 128, 512).transpose(1, 0, 2))
        put("gla_r%d" % cg, _pack_kmajor(gw[:, 2048 + cg * 512:2048 + (cg + 1) * 512]))
        put("gla_o%d" % cg, _pack_kmajor(inp["gla_w_out"][0][:, cg * 512:(cg + 1) * 512]))
    for l in range(2):
        gu = inp["ffn_w_gate_up"][l]
        g4 = _pack_kmajor(gu[:, :DFF])
        u4 = _pack_kmajor(gu[:, DFF:])
        for b in range(11):
            blk = np.stack([np.stack([g4[:, 2 * b + f2], u4[:, 2 * b + f2]], axis=1) for f2 in range(2)], axis=1)
            put("f%d_gu%d" % (l, b), blk)
        wd = inp["ffn_w_down"][l]
        wd4 = wd.reshape(NFC, 128, 8, 128).transpose(1, 2, 0, 3)
        for dc in range(8):
            put("f%d_d%d" % (l, dc), wd4[:, dc])
    dw = inp["gdn_w_in"][0]
    q4 = _pack_kmajor(dw[:, 0:4096])
    for b in range(8):
        put("gdn_qkv%d" % b, q4[:, 4 * b:4 * b + 4])
    z4 = _pack_kmajor(dw[:, 4096:6144])
    for b in range(4):
        put("gdn_z%d" % b, z4[:, 4 * b:4 * b + 4])
    wo = inp["gdn_w_out"][0]
    wo4 = wo.reshape(16, 128, 8, 128).transpose(1, 2, 0, 3)
    for b in range(4):
        put("gdn_o%d" % b, wo4[:, 2 * b:2 * b + 2])
    return wb


def make_consts():
    c = np.zeros((128, NCST), np.float32)
    j = np.arange(128)[:, None]
    i = np.arange(128)[None, :]
    c[:, C_IDENT:C_IDENT + 128] = (i == j)
    c[:, C_MASKT:C_MASKT + 128] = (j <= i)
    c[:, C_TRIS:C_TRIS + 128] = (j <= i) * (-1.0 / 16.0)
    c[:, C_LSTRICT:C_LSTRICT + 128] = (j > i)
    c[:, C_MASKD:C_MASKD + 128] = -1.0 * ((i > j) & ((i // 64) == (j // 64)))
    c[:, C_MASKOFF:C_MASKOFF + 128] = (i >= 64) & (j < 64)
    c[:, C_ONES:C_ONES + 128] = 1.0
    sel = np.zeros((128, 16, 128), np.float32)
    for h in range(16):
        sel[h, h, :] = 1.0
    c[:, C_SEL:] = sel.reshape(128, 2048)
    return c


def make_small(inp):
    s = {}
    nw = np.stack([inp["mix_norm_w"][0], inp["ffn_norm_w"][0], inp["mix_norm_w"][1], inp["ffn_norm_w"][1],
                   inp["final_norm_w"]], axis=0)
    s["nw"] = np.ascontiguousarray(nw.reshape(5, 8, 128).transpose(2, 0, 1))
    gw = inp["gla_w_in"][0]
    s["wglow"] = np.ascontiguousarray(gw[:, 3072:3088].reshape(8, 128, 16).transpose(1, 0, 2))
    s["wgu"] = np.ascontiguousarray(inp["gla_w_gate_up"][0])
    s["bgate"] = np.ascontiguousarray(inp["gla_b_gate"][0].reshape(1, 512))
    s["gnw"] = np.ascontiguousarray(inp["gla_norm_w"][0].reshape(2, 128).T)
    dw = inp["gdn_w_in"][0]
    s["wab"] = np.ascontiguousarray(dw[:, 6144:6176].reshape(8, 128, 32).transpose(1, 0, 2))
    s["cw"] = np.ascontiguousarray(inp["gdn_conv_w"][0].reshape(4, 32, 128).transpose(2, 1, 0))
    s["alog"] = np.ascontiguousarray(inp["gdn_a_log"][0].reshape(1, 16))
    s["dtb"] = np.ascontiguousarray(inp["gdn_dt_bias"][0].reshape(1, 16))
    s["dnw"] = np.ascontiguousarray(inp["gdn_norm_w"][0].reshape(128, 1))
    return s


class Buf:
    __slots__ = ("name", "t", "w", "r", "excl")

    def __init__(self, name, t, excl=False):
        self.name = name; self.t = t; self.w = {}; self.r = {}; self.excl = excl

    def __getitem__(self, k):
        return self.t[k]


def _bk(x):
    if isinstance(x, tuple):
        return x[0], (None if x[0].excl else x[1])
    return x, None


class Prog:
    SAME_ENGINE_SYNC = True

    def __init__(self, nc, es):
        self.nc = nc; self.es = es
        self.eng = {'pe': nc.tensor, 'act': nc.scalar, 'dve': nc.vector, 'pool': nc.gpsimd, 'sp': nc.sync}
        self.sem = {}; self.cnt = {}; self.known = {}
        for k in self.eng:
            self.sem[k] = es.enter_context(nc.semaphore("s_" + k)); self.cnt[k] = 0; self.known[k] = {}
        self.dsems = {}
        self.nwaits = 0; self.ninstr = 0
        self.uid = 0

    def sbuf(self, name, shape, dt, es=None):
        self.uid += 1
        return Buf(name, (es or self.es).enter_context(self.nc.sbuf_tensor("%s_%d" % (name, self.uid), list(shape), dt)))

    def psum(self, name, shape, dt):
        return Buf(name, self.es.enter_context(self.nc.psum_tensor(name, list(shape), dt)), excl=True)

    def _wait(self, e, key, val):
        if self.known[e].get(key, 0) >= val:
            return
        if key == e and (e == 'pe' or not self.SAME_ENGINE_SYNC):
            return
        self.eng[e].wait_ge(self.sem[key], val); self.known[e][key] = val; self.nwaits += 1

    def _deps(self, e, reads, writes):
        for x in reads:
            b, k = _bk(x)
            for wk, tok in b.w.items():
                if k is None or wk is None or wk == k:
                    self._wait(e, *tok)
            if b.excl:
                for rk, d in b.r.items():
                    for en, v in d.items():
                        if en != e:
                            self._wait(e, en, v)
        for x in writes:
            b, k = _bk(x)
            for wk, tok in b.w.items():
                if k is None or wk is None or wk == k:
                    self._wait(e, *tok)
            for rk, d in b.r.items():
                if k is None or rk is None or rk == k:
                    for en, v in d.items():
                        self._wait(e, en, v)

    def _record(self, tok, reads, writes):
        for x in reads:
            b, k = _bk(x)
            d = b.r.setdefault(k, {})
            if d.get(tok[0], 0) < tok[1]:
                d[tok[0]] = tok[1]
        for x in writes:
            b, k = _bk(x)
            if k is None:
                b.w = {None: tok}; b.r = {}
            else:
                b.w[k] = tok; b.r.pop(k, None)

    def op(self, e, reads, writes, fn):
        if e == 'pool' and not DBG.get('usepool'):
            e = 'dve'
        self._deps(e, reads, writes)
        ins = fn(self.eng[e]); self.cnt[e] += 1; ins.then_inc(self.sem[e], 1); self.ninstr += 1
        self._record((e, self.cnt[e]), reads, writes)
        return ins

    def dsem_for(self, key):
        if key not in self.dsems:
            self.dsems[key] = "d_" + key
            self.sem["d_" + key] = self.es.enter_context(self.nc.semaphore("d_" + key))
            self.cnt["d_" + key] = 0
        return "d_" + key

    def dma(self, q, out_buf, out_ap, in_buf, in_ap, semkey=None):
        reads = [in_buf] if in_buf is not None else []
        writes = [out_buf] if out_buf is not None else []
        self._deps(q, reads, writes)
        tgt = out_buf if out_buf is not None else in_buf
        tb = _bk(tgt)[0]
        sk = self.dsem_for(semkey or tb.name)
        ins = self.eng[q].dma_start(out=out_ap, in_=in_ap); self.cnt[sk] += 16
        ins.then_inc(self.sem[sk], 16); self.ninstr += 1
        tok = (sk, self.cnt[sk])
        self._record(tok, reads, writes)
        return tok

    def wait_tok(self, e, tok):
        self._wait(e, *tok)

    def barrier(self, engines=('pe', 'act', 'dve', 'pool')):
        for e in engines:
            for f in ('pe', 'act', 'dve', 'pool'):
                if self.cnt[f] > 0:
                    self._wait(e, f, self.cnt[f])

    def mm(self, ob, oap, lb, lap, rb, rap, start=True, stop=True):
        return self.op('pe', [lb, rb], [ob], lambda g: g.matmul(oap, lap, rap, start=start, stop=stop))


def run_pipeline(makers, width, min_gap=0):
    active = []
    i = 0
    while i < len(makers) or active:
        while (len(active) < width and i < len(makers)
               and (not active or active[-1][1] >= min_gap)):
            gen = makers[i](); i += 1
            try:
                next(gen); active.append([gen, 0])
            except StopIteration:
                pass
        for ent in list(active):
            try:
                next(ent[0]); ent[1] += 1
            except StopIteration:
                active.remove(ent)


def build(n_tiles=8, layers=(0, 1), do_ffn=True, seq=SEQ):
    nc = bass.Bass("TRN2", target_bir_lowering=False)
    dr = {}

    def din(name, shape, dt=F32):
        dr[name] = nc.dram_tensor(name, list(shape), dt, kind="ExternalInput").ap()
        return dr[name]

    xT_d = din("xT", [128, 8, seq])
    wblk_d = din("wblk", [NBLK, 128, BLK])
    cst_d = din("cst", [128, NCST])
    nw_d = din("nw", [128, 5, 8]); wglow_d = din("wglow", [128, 8, 16]); wgu_d = din("wgu", [16, 512])
    bgate_d = din("bgate", [1, 512]); gnw_d = din("gnw", [128, 2]); wab_d = din("wab", [128, 8, 32])
    cw_d = din("cw", [128, 32, 4]); alog_d = din("alog", [1, 16]); dtb_d = din("dtb", [1, 16]); dnw_d = din("dnw", [128, 1])
    out_d = nc.dram_tensor("outT", [128, 8, seq], F32, kind="ExternalOutput").ap()
    wscr = nc.dram_tensor("wscr", [NBLK, 128, BLK], BF16, kind="Internal").ap()

    with ExitStack() as es:
        P = Prog(nc, es)
        hT = P.sbuf("hT", [128, 8, T], F32)
        hnT = P.sbuf("hnT", [128, 8, T], BF16)
        NSLOT = 4
        ring = [P.sbuf("ring%d" % i, [128, BLK], BF16) for i in range(NSLOT)]
        cst = P.sbuf("cst", [128, 896], F32)
        cstb = P.sbuf("cstb", [128, NCST], BF16)
        nw = P.sbuf("nw", [128, 5, 8], F32)
        wglow = P.sbuf("wglow", [128, 8, 16], BF16)
        wgu = P.sbuf("wgu", [16, 512], F32)
        bgate = P.sbuf("bgate", [1, 512], F32)
        gnw = P.sbuf("gnw", [128, 2], F32)
        wab = P.sbuf("wab", [128, 8, 32], BF16)
        cw = P.sbuf("cw", [128, 32, 4], F32)
        negA = P.sbuf("negA", [128, 16], F32)
        dtb = P.sbuf("dtb", [128, 16], F32)
        dnw = P.sbuf("dnw", [128, 1], F32)
        S1 = P.sbuf("S1", [128, 4, 256], F32); S1b = P.sbuf("S1b", [128, 4, 256], BF16)
        S2 = P.sbuf("S2", [128, 16, 128], F32); S2b = P.sbuf("S2b", [128, 16, 128], BF16)
        carry = P.sbuf("carry", [128, 32, 3], F32)
        banks = [P.psum("pb%d" % i, [128, 512], F32) for i in range(7)]
        psb = P.psum("psb", [128, 1024], BF16)
        bstate = [0]

        held = set()

        def bank(hold=False):
            for _ in range(8):
                b = banks[bstate[0] % 7]; bstate[0] += 1
                if b.name not in held:
                    if hold:
                        held.add(b.name)
                    return b
            raise RuntimeError("no free PSUM bank")

        def release(b):
            held.discard(b.name)

        ident = cst[:, C_IDENT:C_IDENT + 128]
        maskT = cst[:, C_MASKT:C_MASKT + 128]
        triS = cst[:, C_TRIS:C_TRIS + 128]
        Lstrict = cst[:, C_LSTRICT:C_LSTRICT + 128]
        maskD = cst[:, C_MASKD:C_MASKD + 128]
        maskOff = cst[:, C_MASKOFF:C_MASKOFF + 128]
        onesf = cst[:, C_ONES:C_ONES + 128]
        identb = cstb[:, C_IDENT:C_IDENT + 128]
        maskTb = cstb[:, C_MASKT:C_MASKT + 128]
        onesb = cstb[:, C_ONES:C_ONES + 128]

        P.dma('sp', cst, cst[:], None, cst_d[:, 0:896])
        P.dma('pool', cstb, cstb[:], None, cst_d)
        P.dma('sp', nw, nw[:], None, nw_d)
        P.dma('pool', wglow, wglow[:], None, wglow_d)
        P.dma('sp', wgu, wgu[:], None, wgu_d)
        P.dma('sp', bgate, bgate[:], None, bgate_d)
        P.dma('sp', gnw, gnw[:], None, gnw_d)
        P.dma('pool', wab, wab[:], None, wab_d)
        P.dma('sp', cw, cw[:], None, cw_d)
        P.dma('sp', negA, negA[:], None, alog_d.partition_broadcast(128))
        P.dma('sp', dtb, dtb[:], None, dtb_d.partition_broadcast(128))
        P.dma('sp', dnw, dnw[:], None, dnw_d)
        P.op('act', [negA], [negA], lambda g: g.activation(out=negA[:], in_=negA[:], func=AF.Exp))
        P.op('dve', [negA], [negA], lambda g: g.tensor_scalar(negA[:], negA[:], -1.0, None, op0=ALU.mult))
        for b_ in (S1, S1b, S2, S2b, carry):
            P.op('dve', [], [b_], lambda g, b_=b_: g.memset(b_[:], 0.0))

        need = []
        if 0 in layers:
            need += [n for n in BLOCKS if n.startswith("gla_")]
            if do_ffn:
                need += [n for n in BLOCKS if n.startswith("f0_")]
        if 1 in layers:
            need += [n for n in BLOCKS if n.startswith("gdn_")]
            if do_ffn:
                need += [n for n in BLOCKS if n.startswith("f1_")]
        NCAST = 8
        castb = [Buf("cast%d" % i, None) for i in range(NCAST)]
        cast_tok = {}
        for n_i, name in enumerate(need):
            bi = BIDX[name]
            cast_tok[name] = P.dma('pool', castb[n_i % NCAST], wscr[bi], None, wblk_d[bi])

        stream = [(t_, name) for t_ in range(n_tiles) for name in need]
        sstate = {"issued": 0, "used": 0}

        def issue_upto(k):
            while sstate["issued"] < min(k, len(stream)):
                i = sstate["issued"]
                t_, name = stream[i]
                slot = ring[i % NSLOT]
                if t_ == 0:
                    P.wait_tok('sp', cast_tok[name])
                P.dma('sp', slot, slot[:], None, wscr[BIDX[name]])
                sstate["issued"] += 1

        def wnext(expect):
            i = sstate["used"]
            assert stream[i][1] == expect, (stream[i], expect)
            issue_upto(i + NSLOT)
            sstate["used"] += 1
            return ring[i % NSLOT]

        nsq = [P.sbuf("nsq%d" % i, [128, T], BF16) for i in range(2)]
        nrstd = P.sbuf("nrstd", [128, T], F32)

        def rmsnorm(widx, out_final=None):
            bk = bank()
            for dc in range(8):
                q_ = nsq[dc % 2]
                P.op('act', [(hT, dc)], [q_], lambda g: g.activation(out=q_[:], in_=hT[:, dc, :], func=AF.Square))
                P.mm(bk, bk[:], cstb, onesb, q_, q_[:], start=(dc == 0), stop=(dc == 7))
            P.op('act', [bk], [nrstd], lambda g: g.activation(out=nrstd[:], in_=bk[:], func=AF.Ln, bias=NORM_EPS, scale=1.0 / D))
            P.op('act', [nrstd], [nrstd], lambda g: g.activation(out=nrstd[:], in_=nrstd[:], func=AF.Exp, scale=-0.5))
            dst = hnT if out_final is None else out_final
            for dc in range(8):
                P.op('dve', [(hT, dc), nw, nrstd], [(dst, dc)], lambda g: g.scalar_tensor_tensor(
                    dst[:, dc, :], hT[:, dc, :], nw[:, widx, dc:dc + 1], nrstd[:], op0=ALU.mult, op1=ALU.mult))

        def ffn(l):
            with ExitStack() as ls:
                actT = P.sbuf("actT", [128, NFC, T], BF16, ls)
                sg = [P.sbuf("sg%d" % i, [128, T], F32, ls) for i in range(2)]
                for b in range(11):
                    slot = wnext("f%d_gu%d" % (l, b))
                    sv = slot[:].rearrange("p (a b k c) -> p a b k c", a=2, b=2, k=8, c=128)
                    for f2 in range(2):
                        f = 2 * b + f2
                        bg = bank(); bu = bank()
                        for kc in range(8):
                            P.mm(bg, bg[:], slot, sv[:, f2, 0, kc, :], hnT, hnT[:, kc, :], start=(kc == 0), stop=(kc == 7))
                        for kc in range(8):
                            P.mm(bu, bu[:], slot, sv[:, f2, 1, kc, :], hnT, hnT[:, kc, :], start=(kc == 0), stop=(kc == 7))
                        s_ = sg[f % 2]
                        P.op('act', [bg], [s_], lambda g: g.activation(out=s_[:], in_=bg[:], func=AF.Silu))
                        P.op('dve', [s_, bu], [(actT, f)], lambda g: g.tensor_tensor(actT[:, f, :], s_[:], bu[:], op=ALU.mult))
                for dc in range(8):
                    slot = wnext("f%d_d%d" % (l, dc))
                    sv = slot[:, 0:DFF].rearrange("p (f c) -> p f c", c=128)
                    bk = bank()
                    for fc in range(NFC):
                        P.mm(bk, bk[:], slot, sv[:, fc, :], (actT, fc), actT[:, fc, :], start=(fc == 0), stop=(fc == NFC - 1))
                    P.op('dve', [(hT, dc), bk], [(hT, dc)], lambda g: g.tensor_tensor(hT[:, dc, :], hT[:, dc, :], bk[:], op=ALU.add))
                P.barrier()

        def gla():
            with ExitStack() as ls:
                glowT = P.sbuf("glowT", [16, T], F32, ls)
                tmpe = [P.sbuf("tmpe%d" % i, [128, 512], F32, ls) for i in range(4)]
                spb = [P.sbuf("spb%d" % i, [128, 512], F32, ls) for i in range(4)]
                EG = P.sbuf("EG", [128, 4, T], F32, ls)
                ENG = P.sbuf("ENG", [128, 4, T], F32, ls)
                qt = P.sbuf("qt", [128, 4, T], BF16, ls)
                kt = P.sbuf("kt", [128, 4, T], BF16, ls)
                vb = P.sbuf("vb", [128, NCH, 1024], BF16, ls)
                sr = P.sbuf("sr", [128, 8, T], BF16, ls)
                scm_all = P.sbuf("scm", [128, NCH, 4, 128], BF16, ls)
                ktok_all = P.sbuf("ktok", [128, NCH, 4, 128], BF16, ls)
                sqo_l = [P.sbuf("sqo%d" % i, [128, 8, 128], BF16, ls) for i in range(2)]
                std_l = [None, None]
                rstd_l = [P.sbuf("grstd%d" % i, [128, 4, 128], F32, ls) for i in range(2)]
                t1_l = [P.sbuf("gt1%d" % i, [128, 8, 128], F32, ls) for i in range(2)]
                bk = bank()
                for kc in range(8):
                    P.mm(bk, bk[0:16, :], wglow, wglow[:, kc, :], hnT, hnT[:, kc, :], start=(kc == 0), stop=(kc == 7))
                P.op('act', [bk], [glowT], lambda g: g.activation(out=glowT[:], in_=bk[0:16, :], func=AF.Copy))
                def chainG(c):
                    cs = slice(c * 128, (c + 1) * 128)
                    te = tmpe[c % 4]; sp_ = spb[c % 4]
                    bx = bank()
                    P.mm(bx, bx[:], glowT, glowT[:, cs], wgu, wgu[:], start=True, stop=False)
                    P.mm(bx, bx[:], cst, onesf[0:1, :], bgate, bgate[:], start=False, stop=True)
                    P.op('act', [bx], [te], lambda g: g.activation(out=te[:], in_=bx[:], func=AF.Exp, scale=-1.0))
                    yield
                    P.op('act', [te], [sp_], lambda g: g.activation(out=sp_[:], in_=te[:], func=AF.Ln, bias=1.0))
                    yield
                    bG = bank()
                    for h in range(4):
                        P.mm(bG, bG[:, h * 128:(h + 1) * 128], sp_, sp_[:, h * 128:(h + 1) * 128], cst, triS)
                    bGv = bG[:].rearrange("p (h c) -> p h c", h=4)
                    P.op('act', [bG], [(EG, c)], lambda g: g.activation(out=EG[:, :, cs], in_=bGv, func=AF.Exp))
                    P.op('act', [bG], [(ENG, c)], lambda g: g.activation(out=ENG[:, :, cs], in_=bGv, func=AF.Exp, scale=-1.0))
                run_pipeline([(lambda c=c: chainG(c)) for c in range(NCH)], 4, min_gap=0)
                slot = wnext("gla_q"); sv = slot[:].rearrange("p (h k c) -> p h k c", h=4, k=8, c=128)
                for h in range(4):
                    bk = bank()
                    for kc in range(8):
                        P.mm(bk, bk[:], slot, sv[:, h, kc, :], hnT, hnT[:, kc, :], start=(kc == 0), stop=(kc == 7))
                    P.op('dve', [bk, EG], [(qt, h)], lambda g: g.scalar_tensor_tensor(
                        qt[:, h, :], bk[:], 128.0 ** -0.5, EG[:, h, :], op0=ALU.mult, op1=ALU.mult))
                slot = wnext("gla_k"); sv = slot[:].rearrange("p (h k c) -> p h k c", h=4, k=8, c=128)
                for h in range(4):
                    bk = bank()
                    for kc in range(8):
                        P.mm(bk, bk[:], slot, sv[:, h, kc, :], hnT, hnT[:, kc, :], start=(kc == 0), stop=(kc == 7))
                    P.op('dve', [bk, ENG], [(kt, h)], lambda g: g.tensor_tensor(kt[:, h, :], bk[:], ENG[:, h, :], op=ALU.mult))
                for cg in range(2):
                    slot = wnext("gla_v%d" % cg); sv = slot[:].rearrange("p (k c) -> p k c", k=8, c=512)
                    for c in range(NCH):
                        bk = bank()
                        for kc in range(8):
                            P.mm(bk, bk[:], hnT, hnT[:, kc, c * 128:(c + 1) * 128], slot, sv[:, kc, :], start=(kc == 0), stop=(kc == 7))
                        P.op('act', [bk], [(vb, (c, cg))], lambda g: g.activation(out=vb[:, c, cg * 512:(cg + 1) * 512], in_=bk[:], func=AF.Copy))
                for cg in range(2):
                    slot = wnext("gla_r%d" % cg); sv = slot[:].rearrange("p (e k c) -> p e k c", e=4, k=8, c=128)
                    for e4 in range(4):
                        bk = bank()
                        for kc in range(8):
                            P.mm(bk, bk[:], slot, sv[:, e4, kc, :], hnT, hnT[:, kc, :], start=(kc == 0), stop=(kc == 7))
                        P.op('act', [bk], [(sr, cg * 4 + e4)], lambda g: g.activation(out=sr[:, cg * 4 + e4, :], in_=bk[:], func=AF.Silu))
                for c in range(NCH):
                    cs = slice(c * 128, (c + 1) * 128)
                    bs = bank()
                    for h in range(4):
                        P.mm(bs, bs[:, h * 128:(h + 1) * 128], (kt, h), kt[:, h, cs], (qt, h), qt[:, h, cs])
                    P.op('dve', [bs, cst], [(scm_all, c)], lambda g: g.tensor_tensor(
                        scm_all[:, c], bs[:].rearrange("p (h c) -> p h c", h=4),
                        maskT.unsqueeze(1).broadcast_to([128, 4, 128]), op=ALU.mult))
                    for h in range(4):
                        P.op('pe', [kt, cstb], [psb], lambda g: g.transpose(psb[:, h * 128:(h + 1) * 128], kt[:, h, cs], identb))
                    P.op('act', [psb], [(ktok_all, c)], lambda g: g.activation(
                        out=ktok_all[:, c], in_=psb[:, 0:512].rearrange("p (h c) -> p h c", h=4), func=AF.Copy))
                def chainC(c):
                    cs = slice(c * 128, (c + 1) * 128)
                    scm = scm_all[:, c]; ktok = ktok_all[:, c]
                    sqo = sqo_l[c % 2]; std = std_l[c % 2]; rstd = rstd_l[c % 2]; t1 = t1_l[c % 2]
                    bo = [bank(hold=True), bank(hold=True)]
                    for h in range(4):
                        for eh in range(2):
                            o_ap = bo[h // 2][:, ((h % 2) * 2 + eh) * 128:((h % 2) * 2 + eh + 1) * 128]
                            P.mm(bo[h // 2], o_ap, vb, vb[:, c, h * 256 + eh * 128:h * 256 + (eh + 1) * 128], (scm_all, c), scm[:, h, :],
                                 start=True, stop=False)
                            P.mm(bo[h // 2], o_ap, S1b, S1b[:, h, eh * 128:(eh + 1) * 128], (qt, h), qt[:, h, cs],
                                 start=False, stop=True)
                    bS = [bank(), bank()]
                    for h in range(4):
                        P.mm(bS[h // 2], bS[h // 2][:, (h % 2) * 256:(h % 2 + 1) * 256], (ktok_all, c), ktok[:, h, :],
                             vb, vb[:, c, h * 256:(h + 1) * 256])
                    for b2 in range(2):
                        P.op('dve', [(S1, b2), bS[b2]], [(S1, b2)], lambda g: g.tensor_tensor(
                            S1[:, 2 * b2:2 * b2 + 2, :], S1[:, 2 * b2:2 * b2 + 2, :],
                            bS[b2][:].rearrange("p (h c) -> p h c", h=2), op=ALU.add))
                    last = c * 128 + 127
                    P.op('dve', [S1, EG], [S1], lambda g: g.tensor_tensor(
                        S1[:], S1[:], EG[:, :, last:last + 1].broadcast_to([128, 4, 256]), op=ALU.mult))
                    P.op('act', [S1], [S1b], lambda g: g.activation(out=S1b[:], in_=S1[:], func=AF.Copy))
                    yield
                    for b2 in range(2):
                        P.op('act', [bo[b2]], [(sqo, b2)], lambda g: g.activation(
                            out=sqo[:, 4 * b2:4 * b2 + 4, :], in_=bo[b2][:].rearrange("p (h c) -> p h c", h=4), func=AF.Square))
                    bq = bank()
                    for h in range(4):
                        for eh in range(2):
                            P.mm(bq, bq[:, h * 128:(h + 1) * 128], cstb, onesb, sqo, sqo[:, 2 * h + eh, :],
                                 start=(eh == 0), stop=(eh == 1))
                    P.op('act', [bq], [rstd], lambda g: g.activation(
                        out=rstd[:], in_=bq[:].rearrange("p (h c) -> p h c", h=4), func=AF.Ln, bias=NORM_EPS, scale=1.0 / 256))
                    P.op('act', [rstd], [rstd], lambda g: g.activation(out=rstd[:], in_=rstd[:], func=AF.Exp, scale=-0.5))
                    yield
                    for b2 in range(2):
                        P.op('dve', [bo[b2], rstd], [(t1, b2)], lambda g: g.tensor_tensor(
                            t1[:, 4 * b2:4 * b2 + 4, :].rearrange("p (h e) c -> p h e c", h=2),
                            bo[b2][:].rearrange("p (h e c) -> p h e c", h=2, e=2),
                            rstd[:, 2 * b2:2 * b2 + 2, :].unsqueeze(2).broadcast_to([128, 2, 2, 128]), op=ALU.mult))
                        release(bo[b2])
                    for eh in range(2):
                        t1v = t1[:].rearrange("p (h e) c -> p h e c", e=2)[:, :, eh, :]
                        srv = sr[:, :, cs].rearrange("p (h e) c -> p h e c", e=2)[:, :, eh, :]
                        P.op('dve', [t1, gnw, sr], [sr], lambda g: g.scalar_tensor_tensor(
                            srv, t1v, gnw[:, eh:eh + 1], srv, op0=ALU.mult, op1=ALU.mult))
                run_pipeline([(lambda c=c: chainC(c)) for c in range(NCH)], 2, min_gap=1)
                for cg in range(2):
                    slot = wnext("gla_o%d" % cg); sv = slot[:].rearrange("p (d k c) -> p d k c", d=4, k=8, c=128)
                    for d4 in range(4):
                        dc = cg * 4 + d4
                        bk = bank()
                        for ec in range(8):
                            P.mm(bk, bk[:], slot, sv[:, d4, ec, :], (sr, ec), sr[:, ec, :], start=(ec == 0), stop=(ec == 7))
                        P.op('dve', [(hT, dc), bk], [(hT, dc)], lambda g: g.tensor_tensor(hT[:, dc, :], hT[:, dc, :], bk[:], op=ALU.add))
                P.barrier()

        def gdn():
            with ExitStack() as ls:
                qn = P.sbuf("qn", [128, 8, T], BF16, ls)
                kn = P.sbuf("kn", [128, 8, T], BF16, ls)
                vtok = P.sbuf("vtok", [128, NCH, 2048], BF16, ls)
                zs = P.sbuf("zs", [128, 16, T], BF16, ls)
                with ExitStack() as la:
                    NA = 5
                    A_ = []
                    for i in range(NA):
                        A_.append(dict(
                            xp=P.sbuf("xpre%d" % i, [128, 3 + T], F32, la), ac=P.sbuf("cacc%d" % i, [128, T], F32, la),
                            sqy=P.sbuf("sqy%d" % i, [128, T], BF16, la), vTc=P.sbuf("vTc%d" % i, [128, T], BF16, la)))
                    ssq_all = P.sbuf("ssq_all", [128, 16, T], F32, la)

                    def chainA(cc, slot, sv, c4, B):
                        xp = B["xp"]; ac = B["ac"]; sqy = B["sqy"]; vTc = B["vTc"]
                        bk = bank()
                        for kc in range(8):
                            P.mm(bk, bk[:], slot, sv[:, c4, kc, :], hnT, hnT[:, kc, :], start=(kc == 0), stop=(kc == 7))
                        CE = 'dve' if DBG.get('carrydve') else 'pool'
                        P.op(CE, [(carry, cc)], [xp], lambda g: g.tensor_copy(xp[:, 0:3], carry[:, cc, :]))
                        P.op('act', [bk], [xp], lambda g: g.activation(out=xp[:, 3:3 + T], in_=bk[:], func=AF.Copy))
                        yield
                        P.op(CE, [xp], [(carry, cc)], lambda g: g.tensor_copy(carry[:, cc, :], xp[:, T:T + 3]))
                        P.op('dve' if DBG.get('tapdve') else 'pool', [xp, cw], [ac], lambda g: g.tensor_scalar(
                            ac[:], xp[:, 3:3 + T], cw[:, cc, 3:4], None, op0=ALU.mult))
                        yield
                        for k in (2, 1, 0):
                            P.op('dve', [xp, cw, ac], [ac], lambda g: g.scalar_tensor_tensor(
                                ac[:], xp[:, k:k + T], cw[:, cc, k:k + 1], ac[:], op0=ALU.mult, op1=ALU.add))
                        yield
                        if cc < 16:
                            dst = qn if cc < 8 else kn
                            P.op('act', [ac], [(dst, cc % 8)], lambda g: g.activation(out=dst[:, cc % 8, :], in_=ac[:], func=AF.Silu))
                            P.op('act', [(dst, cc % 8)], [sqy], lambda g: g.activation(out=sqy[:], in_=dst[:, cc % 8, :], func=AF.Square))
                            yield
                            bq = bank()
                            P.mm(bq, bq[:], cstb, onesb, sqy, sqy[:])
                            P.op('act', [bq], [(ssq_all, cc)], lambda g: g.activation(out=ssq_all[:, cc, :], in_=bq[:], func=AF.Copy))
                        else:
                            P.op('act', [ac], [vTc], lambda g: g.activation(out=vTc[:], in_=ac[:], func=AF.Silu))
                            yield
                            for c in range(NCH):
                                P.op('pe', [vTc, cstb], [psb], lambda g: g.transpose(
                                    psb[:, c * 128:(c + 1) * 128], vTc[:, c * 128:(c + 1) * 128], identb))
                            vc = cc - 16
                            P.op('dve', [psb], [(vtok, vc)], lambda g: g.tensor_copy(
                                vtok[:, :, vc * 128:(vc + 1) * 128], psb[:, 0:512].rearrange("p (c e) -> p c e", c=NCH)))

                    cur = {}

                    def mkA(cc):
                        def f():
                            if cc % 4 == 0:
                                cur["slot"] = wnext("gdn_qkv%d" % (cc // 4))
                                cur["sv"] = cur["slot"][:].rearrange("p (a k c) -> p a k c", a=4, k=8, c=128)
                            return chainA(cc, cur["slot"], cur["sv"], cc % 4, A_[cc % NA])
                        return f
                    run_pipeline([mkA(cc) for cc in range(32)], NA, min_gap=1)
                    for b in range(4):
                        slot = wnext("gdn_z%d" % b); sv = slot[:].rearrange("p (a k c) -> p a k c", a=4, k=8, c=128)
                        for c4 in range(4):
                            bk = bank()
                            for kc in range(8):
                                P.mm(bk, bk[:], slot, sv[:, c4, kc, :], hnT, hnT[:, kc, :], start=(kc == 0), stop=(kc == 7))
                            P.op('act', [bk], [(zs, 4 * b + c4)], lambda g: g.activation(out=zs[:, 4 * b + c4, :], in_=bk[:], func=AF.Silu))
                    import math
                    for qi, dst in enumerate((qn, kn)):
                        sl = ssq_all[:, 8 * qi:8 * qi + 8, :]
                        P.op('act', [ssq_all], [(ssq_all, ('h', qi))], lambda g: g.activation(out=sl, in_=sl, func=AF.Ln, bias=L2_EPS, scale=1.0))
                        P.op('act', [ssq_all], [(ssq_all, ('h', qi))], lambda g: g.activation(
                            out=sl, in_=sl, func=AF.Exp, scale=-0.5, bias=(-0.5 * math.log(128.0) if qi == 0 else 0.0)))
                        P.op('dve', [ssq_all, dst], [dst], lambda g: g.tensor_tensor(dst[:], dst[:], sl, op=ALU.mult))
                    P.barrier()
                with ExitStack() as lb:
                    def sb(name, shape, dt):
                        return P.sbuf(name, list(shape), dt, lb)
                    pc = []
                    for par in range(2):
                        d_ = {}
                        for nm in ("beta", "lnbeta", "tz", "gg", "Gcol", "negEG", "dcol", "bd", "EGL"):
                            d_[nm] = sb(nm + str(par), (128, 16), F32)
                        d_["eGT"] = sb("eGT" + str(par), (16, 128), BF16)
                        d_["ktk"] = sb("ktk" + str(par), (128, 8, 128), BF16)
                        pc.append(d_)
                    sets = []
                    for si in range(NSETS):
                        d_ = {}
                        for nm in ("KKD", "KKO", "QKm"):
                            d_[nm] = sb(nm + str(si), (128, 2, 128), F32)
                        for nm in ("lhs4", "bdec"):
                            d_[nm] = sb(nm + str(si), (128, 4, 128), F32)
                        for nm in ("NTa", "NTb", "Na", "Nb", "TTa", "TTb", "MTo", "x0", "attnT", "qg", "vdec"):
                            d_[nm] = sb(nm + str(si), (128, 4, 128), BF16)
                        d_["x1"] = d_["NTa"]; d_["x2"] = d_["NTb"]; d_["sqo"] = d_["Na"]; d_["vpre"] = d_["Nb"]
                        sets.append(d_)
                    v4 = lambda ap: ap.rearrange("p (h c) -> p h c", h=4)
                    selb = cstb[0:16, C_SELB:C_SELB + 2048].rearrange("p (h c) -> p h c", h=16)

                    def preamble(c):
                        q_ = pc[c % 2]
                        beta = q_["beta"]; lnbeta = q_["lnbeta"]; tz = q_["tz"]; gg = q_["gg"]; Gcol = q_["Gcol"]
                        negEG = q_["negEG"]; dcol = q_["dcol"]; bd = q_["bd"]; EGL = q_["EGL"]; eGT = q_["eGT"]; ktk = q_["ktk"]
                        cs = slice(c * 128, (c + 1) * 128)
                        bab = bank()
                        for kc in range(8):
                            P.mm(bab, bab[:, 0:32], hnT, hnT[:, kc, cs], wab, wab[:, kc, :], start=(kc == 0), stop=(kc == 7))
                        P.op('act', [bab], [lnbeta], lambda g: g.activation(out=lnbeta[:], in_=bab[:, 0:16], func=AF.Exp, scale=-1.0))
                        P.op('dve', [bab, dtb], [tz], lambda g: g.tensor_tensor(tz[:], bab[:, 16:32], dtb[:], op=ALU.add))
                        P.op('act', [lnbeta], [lnbeta], lambda g: g.activation(out=lnbeta[:], in_=lnbeta[:], func=AF.Ln, bias=1.0))
                        P.op('pool', [lnbeta], [lnbeta], lambda g: g.tensor_scalar(lnbeta[:], lnbeta[:], -1.0, None, op0=ALU.mult))
                        P.op('act', [lnbeta], [beta], lambda g: g.activation(out=beta[:], in_=lnbeta[:], func=AF.Exp))
                        P.op('act', [tz], [tz], lambda g: g.activation(out=tz[:], in_=tz[:], func=AF.Exp))
                        P.op('act', [tz], [tz], lambda g: g.activation(out=tz[:], in_=tz[:], func=AF.Ln, bias=1.0))
                        P.op('dve', [tz, negA], [gg], lambda g: g.tensor_tensor(gg[:], tz[:], negA[:], op=ALU.mult))
                        bG = bank()
                        P.mm(bG, bG[:, 0:16], cst, maskT, gg, gg[:])
                        P.mm(bG, bG[:, 16:32], cst, onesf, gg, gg[:])
                        P.mm(bG, bG[0:16, 128:256], gg, gg[:], cst, maskT)
                        P.op('act', [bG], [Gcol], lambda g: g.activation(out=Gcol[:], in_=bG[:, 0:16], func=AF.Copy))
                        P.op('act', [bG], [negEG], lambda g: g.activation(out=negEG[:], in_=bG[:, 0:16], func=AF.Exp))
                        P.op('act', [bG], [EGL], lambda g: g.activation(out=EGL[:], in_=bG[:, 16:32], func=AF.Exp))
                        P.op('act', [bG], [eGT], lambda g: g.activation(out=eGT[:], in_=bG[0:16, 128:256], func=AF.Exp))
                        P.op('dve', [bG, Gcol], [dcol], lambda g: g.tensor_tensor(dcol[:], bG[:, 16:32], Gcol[:], op=ALU.subtract))
                        P.op('pool', [negEG], [negEG], lambda g: g.tensor_scalar(negEG[:], negEG[:], -1.0, None, op0=ALU.mult))
                        P.op('act', [dcol], [dcol], lambda g: g.activation(out=dcol[:], in_=dcol[:], func=AF.Exp))
                        P.op('pool', [dcol, beta], [bd], lambda g: g.tensor_tensor(bd[:], dcol[:], beta[:], op=ALU.mult))
                        for gq in range(8):
                            P.op('pe', [(kn, gq), cstb], [psb], lambda g: g.transpose(psb[:, gq * 128:(gq + 1) * 128], kn[:, gq, cs], identb))
                        P.op('act', [psb], [ktk], lambda g: g.activation(
                            out=ktk[:], in_=psb[:].rearrange("p (h c) -> p h c", h=8), func=AF.Copy))

                    def chain(c, grp, s_):
                        q_ = pc[c % 2]
                        lnbeta = q_["lnbeta"]; gg = q_["gg"]
                        negEG = q_["negEG"]; bd = q_["bd"]; EGL = q_["EGL"]; eGT = q_["eGT"]; ktk = q_["ktk"]
                        KKD = s_["KKD"]; KKO = s_["KKO"]; QKm = s_["QKm"]; lhs4 = s_["lhs4"]; bdec = s_["bdec"]
                        MTo = s_["MTo"]; x0 = s_["x0"]; x1 = s_["x1"]; x2 = s_["x2"]
                        attnT = s_["attnT"]; qg = s_["qg"]; vpre = s_["vpre"]; vdec = s_["vdec"]; sqo = s_["sqo"]
                        rstd = s_["lhs4"]
                        NTl = [s_["NTa"], s_["NTb"]]; Nl = [s_["Na"], s_["Nb"]]; TTl = [s_["TTa"], s_["TTb"]]
                        cs = slice(c * 128, (c + 1) * 128)
                        h0 = 4 * grp; g0 = 2 * grp
                        P.op('pool', [cst, gg], [lhs4], lambda g: g.tensor_tensor(
                            lhs4[:], Lstrict.unsqueeze(1).broadcast_to([128, 4, 128]),
                            gg[:, h0:h0 + 4].unsqueeze(2).broadcast_to([128, 4, 128]), op=ALU.mult))
                        bkk = bank()
                        for gi in range(2):
                            P.mm(bkk, bkk[:, gi * 128:(gi + 1) * 128], (kn, g0 + gi), kn[:, g0 + gi, cs], (kn, g0 + gi), kn[:, g0 + gi, cs])
                            P.mm(bkk, bkk[:, (2 + gi) * 128:(3 + gi) * 128], (kn, g0 + gi), kn[:, g0 + gi, cs], (qn, g0 + gi), qn[:, g0 + gi, cs])
                        kk2 = bkk[:, 0:256].rearrange("p (h c) -> p h c", h=2)
                        P.op('dve', [bkk, cst], [KKD], lambda g: g.tensor_tensor(
                            KKD[:], kk2, maskD.unsqueeze(1).broadcast_to([128, 2, 128]), op=ALU.mult))
                        P.op('dve', [bkk, cst], [KKO], lambda g: g.tensor_tensor(
                            KKO[:], kk2, maskOff.unsqueeze(1).broadcast_to([128, 2, 128]), op=ALU.mult))
                        P.op('dve', [bkk, cst], [QKm], lambda g: g.tensor_tensor(
                            QKm[:], bkk[:, 256:512].rearrange("p (h c) -> p h c", h=2),
                            maskT.unsqueeze(1).broadcast_to([128, 2, 128]), op=ALU.mult))
                        yield
                        bd_ = bank()
                        for hh in range(4):
                            P.mm(bd_, bd_[:, hh * 128:(hh + 1) * 128], lhs4, lhs4[:, hh, :], cst, maskT)
                        for hh in range(4):
                            P.op('act', [bd_, lnbeta], [(bdec, hh)], lambda g: g.activation(
                                out=bdec[:, hh, :], in_=bd_[:, hh * 128:(hh + 1) * 128], func=AF.Exp,
                                bias=lnbeta[:, h0 + hh:h0 + hh + 1], scale=1.0))
                        bks = bank()
                        for hh in range(4):
                            P.mm(bks, bks[:, hh * 128:(hh + 1) * 128], (kn, (h0 + hh) // 2), kn[:, (h0 + hh) // 2, cs], (S2b, grp), S2b[:, h0 + hh, :])
                        for hh in range(4):
                            h = h0 + hh
                            P.op('dve', [bks, negEG, (vtok, h)], [(x0, hh)], lambda g: g.scalar_tensor_tensor(
                                x0[:, hh, :], bks[:, hh * 128:(hh + 1) * 128], negEG[:, h:h + 1],
                                vtok[:, c, h * 128:(h + 1) * 128], op0=ALU.mult, op1=ALU.add))
                        bq = bank()
                        for hh in range(4):
                            P.mm(bq, bq[:, hh * 128:(hh + 1) * 128], cstb, selb[:, h0 + hh, :], eGT, eGT[:])
                        P.op('dve', [bq, qn], [qg], lambda g: g.tensor_tensor(
                            qg[:].rearrange("p (a b) c -> p a b c", a=2),
                            bq[:].rearrange("p (a b c) -> p a b c", a=2, b=2),
                            qn[:, g0:g0 + 2, cs].unsqueeze(2).broadcast_to([128, 2, 2, 128]), op=ALU.mult))
                        yield
                        NT = NTl[0]; N_ = Nl[0]
                        pair = lambda b_: b_[:].unsqueeze(2).broadcast_to([128, 2, 2, 128])
                        r4 = lambda b_: b_[:].rearrange("p (a b) c -> p a b c", a=2)
                        P.op('dve', [KKD, bdec], [NT], lambda g: g.tensor_tensor(r4(NT), r4(bdec), pair(KKD), op=ALU.mult))
                        for hh in range(4):
                            P.op('pe', [NT, cstb], [psb], lambda g: g.transpose(psb[:, hh * 128:(hh + 1) * 128], NT[:, hh, :], identb))
                        P.op('act', [psb], [N_], lambda g: g.activation(out=N_[:], in_=v4(psb[:, 0:512]), func=AF.Copy))
                        TT = TTl[0]
                        P.op('pool', [NT, cstb], [TT], lambda g: g.tensor_tensor(
                            TT[:], NT[:], identb.unsqueeze(1).broadcast_to([128, 4, 128]), op=ALU.add))
                        P.op('dve', [KKO, bdec], [MTo], lambda g: g.tensor_tensor(r4(MTo), r4(bdec), pair(KKO), op=ALU.mult))
                        P.op('dve', [QKm, bdec], [attnT], lambda g: g.tensor_tensor(r4(attnT), r4(bdec), pair(QKm), op=ALU.mult))
                        yield
                        for lvl in range(5):
                            NTc = NTl[lvl % 2]; Nc = Nl[lvl % 2]
                            NTn = NTl[(lvl + 1) % 2]; Nn = Nl[(lvl + 1) % 2]
                            TTo = TTl[lvl % 2]; TTn = TTl[(lvl + 1) % 2]
                            bN = bank()
                            for hh in range(4):
                                P.mm(bN, bN[:, hh * 128:(hh + 1) * 128], NTc, NTc[:, hh, :], Nc, Nc[:, hh, :])
                            if lvl < 4:
                                bNT = bank()
                                for hh in range(4):
                                    P.mm(bNT, bNT[:, hh * 128:(hh + 1) * 128], Nc, Nc[:, hh, :], NTc, NTc[:, hh, :])
                            P.op('act', [bN], [Nn], lambda g: g.activation(out=Nn[:], in_=v4(bN[:]), func=AF.Copy))
                            if lvl < 4:
                                P.op('dve', [bNT], [NTn], lambda g: g.tensor_copy(NTn[:], v4(bNT[:])))
                            yield
                            bT = bank()
                            for hh in range(4):
                                o_ap = bT[:, hh * 128:(hh + 1) * 128]
                                P.mm(bT, o_ap, Nn, Nn[:, hh, :], TTo, TTo[:, hh, :], start=True, stop=False)
                                P.mm(bT, o_ap, cstb, identb, TTo, TTo[:, hh, :], start=False, stop=True)
                            if lvl % 2 == 0:
                                P.op('act', [bT], [TTn], lambda g: g.activation(out=TTn[:], in_=v4(bT[:]), func=AF.Copy))
                            else:
                                P.op('dve', [bT], [TTn], lambda g: g.tensor_copy(TTn[:], v4(bT[:])))
                            yield
                        TT = TTl[5 % 2]
                        b1 = bank()
                        for hh in range(4):
                            P.mm(b1, b1[:, hh * 128:(hh + 1) * 128], TT, TT[:, hh, :], (x0, hh), x0[:, hh, :])
                        P.op('act', [b1], [x1], lambda g: g.activation(out=x1[:], in_=v4(b1[:]), func=AF.Copy))
                        yield
                        b2 = bank()
                        for hh in range(4):
                            P.mm(b2, b2[:, hh * 128:(hh + 1) * 128], MTo, MTo[:, hh, :], x1, x1[:, hh, :])
                        P.op('dve', [b2, x0], [x2], lambda g: g.tensor_tensor(x2[:], x0[:], v4(b2[:]), op=ALU.subtract))
                        yield
                        b3 = bank()
                        for hh in range(4):
                            P.mm(b3, b3[:, hh * 128:(hh + 1) * 128], TT, TT[:, hh, :], x2, x2[:, hh, :])
                        P.op('act', [b3], [vpre], lambda g: g.activation(out=vpre[:], in_=v4(b3[:]), func=AF.Copy))
                        P.op('dve', [b3, bd], [vdec], lambda g: g.tensor_tensor(
                            vdec[:], v4(b3[:]), bd[:, h0:h0 + 4].unsqueeze(2).broadcast_to([128, 4, 128]), op=ALU.mult))
                        yield
                        bo = bank(hold=True)
                        for hh in range(4):
                            o_ap = bo[:, hh * 128:(hh + 1) * 128]
                            P.mm(bo, o_ap, vpre, vpre[:, hh, :], attnT, attnT[:, hh, :], start=True, stop=False)
                            P.mm(bo, o_ap, (S2b, grp), S2b[:, h0 + hh, :], qg, qg[:, hh, :], start=False, stop=True)
                        bS = bank()
                        for hh in range(4):
                            P.mm(bS, bS[:, hh * 128:(hh + 1) * 128], ktk, ktk[:, (h0 + hh) // 2, :], vdec, vdec[:, hh, :])
                        P.op('pool', [(S2, grp), EGL], [(S2, grp)], lambda g: g.tensor_tensor(
                            S2[:, h0:h0 + 4, :], S2[:, h0:h0 + 4, :],
                            EGL[:, h0:h0 + 4].unsqueeze(2).broadcast_to([128, 4, 128]), op=ALU.mult))
                        P.op('act', [bo], [sqo], lambda g: g.activation(out=sqo[:], in_=v4(bo[:]), func=AF.Square))
                        P.op('dve', [(S2, grp), bS], [(S2, grp)], lambda g: g.tensor_tensor(
                            S2[:, h0:h0 + 4, :], S2[:, h0:h0 + 4, :], v4(bS[:]), op=ALU.add))
                        P.op('act', [(S2, grp)], [(S2b, grp)], lambda g: g.activation(out=S2b[:, h0:h0 + 4, :], in_=S2[:, h0:h0 + 4, :], func=AF.Copy))
                        yield
                        bn = bank()
                        P.mm(bn, bn[:], cstb, onesb, sqo, sqo[:].rearrange("p h c -> p (h c)"))
                        P.op('act', [bn], [rstd], lambda g: g.activation(out=rstd[:], in_=v4(bn[:]), func=AF.Ln, bias=NORM_EPS, scale=1.0 / 128))
                        P.op('act', [rstd], [rstd], lambda g: g.activation(out=rstd[:], in_=rstd[:], func=AF.Exp, scale=-0.5))
                        yield
                        P.op('dve', [rstd] + [(zs, h0 + i_) for i_ in range(4)], [(zs, h0 + i_) for i_ in range(4)], lambda g: g.tensor_tensor(
                            zs[:, h0:h0 + 4, cs], zs[:, h0:h0 + 4, cs], rstd[:], op=ALU.mult))
                        P.op('dve', [bo, dnw] + [(zs, h0 + i_) for i_ in range(4)], [(zs, h0 + i_) for i_ in range(4)], lambda g: g.scalar_tensor_tensor(
                            zs[:, h0:h0 + 4, cs], v4(bo[:]), dnw[:, 0:1], zs[:, h0:h0 + 4, cs], op0=ALU.mult, op1=ALU.mult))
                        release(bo)

                    def mkB(ti):
                        def f():
                            c_, grp_ = ti // 4, ti % 4
                            if grp_ == 0:
                                preamble(c_)
                            return chain(c_, grp_, sets[ti % NSETS])
                        return f
                    run_pipeline([mkB(ti) for ti in range(NCH * 4)], NSETS, min_gap=B_GAP)
                    P.barrier()
                for b in range(4):
                    slot = wnext("gdn_o%d" % b); sv = slot[:].rearrange("p (d k c) -> p d k c", d=2, k=16, c=128)
                    for d2 in range(2):
                        dc = 2 * b + d2
                        bk = bank()
                        for ec in range(16):
                            P.mm(bk, bk[:], slot, sv[:, d2, ec, :], (zs, ec), zs[:, ec, :], start=(ec == 0), stop=(ec == 15))
                        P.op('dve', [(hT, dc), bk], [(hT, dc)], lambda g: g.tensor_tensor(hT[:, dc, :], hT[:, dc, :], bk[:], op=ALU.add))
                P.barrier()

        for t_ in range(n_tiles):
            ts = slice(t_ * T, (t_ + 1) * T)
            for dc_ in range(8):
                P.dma('sp', (hT, dc_), hT[:, dc_, :], None, xT_d[:, dc_, ts], semkey='hT%d' % dc_)
            issue_upto(NSLOT)
            if 0 in layers:
                rmsnorm(0); gla()
                if do_ffn:
                    rmsnorm(1); ffn(0)
            if 1 in layers:
                rmsnorm(2); gdn()
                if do_ffn:
                    rmsnorm(3); ffn(1)
            with ExitStack() as lo:
                oT = P.sbuf("oT", [128, 8, T], F32, lo)
                rmsnorm(4, out_final=oT)
                for dc_ in range(8):
                    last_tok = P.dma('sp', None, out_d[:, dc_, ts], (oT, dc_), oT[:, dc_, :], semkey="outst")
                for e_ in ('pe', 'act', 'dve', 'pool'):
                    P.wait_tok(e_, last_tok)
        P.wait_tok('sp', last_tok)
        P.barrier()
        build.stats = (P.ninstr, P.nwaits, dict(P.cnt))
    return nc


def prep_inputs(inp, n_cores=8, seq=SEQ):
    wb = make_wblocks(inp)
    cst = make_consts()
    sm = make_small(inp)
    maps = []
    for b in range(n_cores):
        xT = np.ascontiguousarray(inp["x"][b, :seq].T.reshape(8, 128, seq).transpose(1, 0, 2))
        m = {"xT": xT, "wblk": wb, "cst": cst}
        m.update(sm)
        maps.append(m)
    return maps


def kernel(**inputs):
    inp = {k: np.asarray(v, dtype=np.float32) for k, v in inputs.items()}
    nc = build()
    maps = prep_inputs(inp)
    res = run_bass_kernel_spmd(nc, maps, core_ids=list(range(8)))
    outs = []
    for b in range(8):
        oT = res.results[b]["outT"]
        outs.append(oT.transpose(2, 1, 0).reshape(SEQ, D))
    return np.stack(outs, axis=0).astype(np.float32)
```

```python
import numpy as np
from contextlib import ExitStack
import concourse.bass as bass
import concourse.mybir as mybir
from concourse.bass_utils import run_bass_kernel_spmd

F32 = mybir.dt.float32
BF16 = mybir.dt.bfloat16
AF = mybir.ActivationFunctionType
ALU = mybir.AluOpType
AX = mybir.AxisListType

D = 1024
SEQ = 4096
T = 512
NCH = T // 128
DFF = 2816
NFC = DFF // 128
BLK = 4096
NORM_EPS = 1e-6
L2_EPS = 1e-6
NEUMANN_BF16 = True
NSETS = 3
B_GAP = 3
import os
DBG = {k: True for k in os.environ.get('KDBG', '').split(',') if k}

BLOCKS = (["gla_q", "gla_k", "gla_v0", "gla_v1", "gla_r0", "gla_r1", "gla_o0", "gla_o1"]
          + ["f0_gu%d" % i for i in range(11)] + ["f0_d%d" % i for i in range(8)]
          + ["gdn_qkv%d" % i for i in range(8)] + ["gdn_z%d" % i for i in range(4)]
          + ["gdn_o%d" % i for i in range(4)]
          + ["f1_gu%d" % i for i in range(11)] + ["f1_d%d" % i for i in range(8)])
NBLK = len(BLOCKS)
BIDX = {n: i for i, n in enumerate(BLOCKS)}

C_IDENT, C_MASKT, C_TRIS, C_LSTRICT, C_MASKD, C_MASKOFF, C_ONES, C_SEL = 0, 128, 256, 384, 512, 640, 768, 896
NCST = 896 + 2048
C_SELB = 896


def _pack_kmajor(W):
    nc_ = W.shape[1] // 128
    return W.reshape(8, 128, nc_, 128).transpose(1, 2, 0, 3)


def make_wblocks(inp):
    wb = np.zeros((NBLK, 128, BLK), np.float32)

    def put(name, arr):
        a = np.ascontiguousarray(arr).reshape(128, -1)
        wb[BIDX[name], :, :a.shape[1]] = a

    gw = inp["gla_w_in"][0]
    put("gla_q", _pack_kmajor(gw[:, 0:512]))
    put("gla_k", _pack_kmajor(gw[:, 512:1024]))
    for cg in range(2):
        put("gla_v%d" % cg, gw[:, 1024 + cg * 512:1024 + (cg + 1) * 512].reshape(8, 128, 512).transpose(1, 0, 2))
        put("gla_r%d" % cg, _pack_kmajor(gw[:, 2048 + cg * 512:2048 + (cg + 1) * 512]))
        put("gla_o%d" % cg, _pack_kmajor(inp["gla_w_out"][0][:, cg * 512:(cg + 1) * 512]))
    for l in range(2):
        gu = inp["ffn_w_gate_up"][l]
        g4 = _pack_kmajor(gu[:, :DFF])
        u4 = _pack_kmajor(gu[:, DFF:])
        for b in range(11):
            blk = np.stack([np.stack([g4[:, 2 * b + f2], u4[:, 2 * b + f2]], axis=1) for f2 in range(2)], axis=1)
            put("f%d_gu%d" % (l, b), blk)
        wd = inp["ffn_w_down"][l]
        wd4 = wd.reshape(NFC, 128, 8, 128).transpose(1, 2, 0, 3)
        for dc in range(8):
            put("f%d_d%d" % (l, dc), wd4[:, dc])
    dw = inp["gdn_w_in"][0]
    q4 = _pack_kmajor(dw[:, 0:4096])
    for b in range(8):
        put("gdn_qkv%d" % b, q4[:, 4 * b:4 * b + 4])
    z4 = _pack_kmajor(dw[:, 4096:6144])
    for b in range(4):
        put("gdn_z%d" % b, z4[:, 4 * b:4 * b + 4])
    wo = inp["gdn_w_out"][0]
    wo4 = wo.reshape(16, 128, 8, 128).transpose(1, 2, 0, 3)
    for b in range(4):
        put("gdn_o%d" % b, wo4[:, 2 * b:2 * b + 2])
    return wb


def make_consts():
    c = np.zeros((128, NCST), np.float32)
    j = np.arange(128)[:, None]
    i = np.arange(128)[None, :]
    c[:, C_IDENT:C_IDENT + 128] = (i == j)
    c[:, C_MASKT:C_MASKT + 128] = (j <= i)
    c[:, C_TRIS:C_TRIS + 128] = (j <= i) * (-1.0 / 16.0)
    c[:, C_LSTRICT:C_LSTRICT + 128] = (j > i)
    c[:, C_MASKD:C_MASKD + 128] = -1.0 * ((i > j) & ((i // 64) == (j // 64)))
    c[:, C_MASKOFF:C_MASKOFF + 128] = (i >= 64) & (j < 64)
    c[:, C_ONES:C_ONES + 128] = 1.0
    sel = np.zeros((128, 16, 128), np.float32)
    for h in range(16):
        sel[h, h, :] = 1.0
    c[:, C_SEL:] = sel.reshape(128, 2048)
    return c


def make_small(inp):
    s = {}
    nw = np.stack([inp["mix_norm_w"][0], inp["ffn_norm_w"][0], inp["mix_norm_w"][1], inp["ffn_norm_w"][1],
                   inp["final_norm_w"]], axis=0)
    s["nw"] = np.ascontiguousarray(nw.reshape(5, 8, 128).transpose(2, 0, 1))
    gw = inp["gla_w_in"][0]
    s["wglow"] = np.ascontiguousarray(gw[:, 3072:3088].reshape(8, 128, 16).transpose(1, 0, 2))
    s["wgu"] = np.ascontiguousarray(inp["gla_w_gate_up"][0])
    s["bgate"] = np.ascontiguousarray(inp["gla_b_gate"][0].reshape(1, 512))
    s["gnw"] = np.ascontiguousarray(inp["gla_norm_w"][0].reshape(2, 128).T)
    dw = inp["gdn_w_in"][0]
    s["wab"] = np.ascontiguousarray(dw[:, 6144:6176].reshape(8, 128, 32).transpose(1, 0, 2))
    s["cw"] = np.ascontiguousarray(inp["gdn_conv_w"][0].reshape(4, 32, 128).transpose(2, 1, 0))
    s["alog"] = np.ascontiguousarray(inp["gdn_a_log"][0].reshape(1, 16))
    s["dtb"] = np.ascontiguousarray(inp["gdn_dt_bias"][0].reshape(1, 16))
    s["dnw"] = np.ascontiguousarray(inp["gdn_norm_w"][0].reshape(128, 1))
    return s


class Buf:
    __slots__ = ("name", "t", "w", "r", "excl")

    def __init__(self, name, t, excl=False):
        self.name = name; self.t = t; self.w = {}; self.r = {}; self.excl = excl

    def __getitem__(self, k):
        return self.t[k]


def _bk(x):
    if isinstance(x, tuple):
        return x[0], (None if x[0].excl else x[1])
    return x, None


class Prog:
    SAME_ENGINE_SYNC = True

    def __init__(self, nc, es):
        self.nc = nc; self.es = es
        self.eng = {'pe': nc.tensor, 'act': nc.scalar, 'dve': nc.vector, 'pool': nc.gpsimd, 'sp': nc.sync}
        self.sem = {}; self.cnt = {}; self.known = {}
        for k in self.eng:
            self.sem[k] = es.enter_context(nc.semaphore("s_" + k)); self.cnt[k] = 0; self.known[k] = {}
        self.dsems = {}
        self.nwaits = 0; self.ninstr = 0
        self.uid = 0

    def sbuf(self, name, shape, dt, es=None):
        self.uid += 1
        return Buf(name, (es or self.es).enter_context(self.nc.sbuf_tensor("%s_%d" % (name, self.uid), list(shape), dt)))

    def psum(self, name, shape, dt):
        return Buf(name, self.es.enter_context(self.nc.psum_tensor(name, list(shape), dt)), excl=True)

    def _wait(self, e, key, val):
        if self.known[e].get(key, 0) >= val:
            return
        if key == e and (e == 'pe' or not self.SAME_ENGINE_SYNC):
            return
        self.eng[e].wait_ge(self.sem[key], val); self.known[e][key] = val; self.nwaits += 1

    def _deps(self, e, reads, writes):
        for x in reads:
            b, k = _bk(x)
            for wk, tok in b.w.items():
                if k is None or wk is None or wk == k:
                    self._wait(e, *tok)
            if b.excl:
                for rk, d in b.r.items():
                    for en, v in d.items():
                        if en != e:
                            self._wait(e, en, v)
        for x in writes:
            b, k = _bk(x)
            for wk, tok in b.w.items():
                if k is None or wk is None or wk == k:
                    self._wait(e, *tok)
            for rk, d in b.r.items():
                if k is None or rk is None or rk == k:
                    for en, v in d.items():
                        self._wait(e, en, v)

    def _record(self, tok, reads, writes):
        for x in reads:
            b, k = _bk(x)
            d = b.r.setdefault(k, {})
            if d.get(tok[0], 0) < tok[1]:
                d[tok[0]] = tok[1]
        for x in writes:
            b, k = _bk(x)
            if k is None:
                b.w = {None: tok}; b.r = {}
            else:
                b.w[k] = tok; b.r.pop(k, None)

    def op(self, e, reads, writes, fn):
        if e == 'pool' and not DBG.get('usepool'):
            e = 'dve'
        self._deps(e, reads, writes)
        ins = fn(self.eng[e]); self.cnt[e] += 1; ins.then_inc(self.sem[e], 1); self.ninstr += 1
        self._record((e, self.cnt[e]), reads, writes)
        return ins

    def dsem_for(self, key):
        if key not in self.dsems:
            self.dsems[key] = "d_" + key
            self.sem["d_" + key] = self.es.enter_context(self.nc.semaphore("d_" + key))
            self.cnt["d_" + key] = 0
        return "d_" + key

    def dma(self, q, out_buf, out_ap, in_buf, in_ap, semkey=None):
        reads = [in_buf] if in_buf is not None else []
        writes = [out_buf] if out_buf is not None else []
        self._deps(q, reads, writes)
        tgt = out_buf if out_buf is not None else in_buf
        tb = _bk(tgt)[0]
        sk = self.dsem_for(semkey or tb.name)
        ins = self.eng[q].dma_start(out=out_ap, in_=in_ap); self.cnt[sk] += 16
        ins.then_inc(self.sem[sk], 16); self.ninstr += 1
        tok = (sk, self.cnt[sk])
        self._record(tok, reads, writes)
        return tok

    def wait_tok(self, e, tok):
        self._wait(e, *tok)

    def barrier(self, engines=('pe', 'act', 'dve', 'pool')):
        for e in engines:
            for f in ('pe', 'act', 'dve', 'pool'):
                if self.cnt[f] > 0:
                    self._wait(e, f, self.cnt[f])

    def mm(self, ob, oap, lb, lap, rb, rap, start=True, stop=True):
        return self.op('pe', [lb, rb], [ob], lambda g: g.matmul(oap, lap, rap, start=start, stop=stop))


def run_pipeline(makers, width, min_gap=0):
    active = []
    i = 0
    while i < len(makers) or active:
        while (len(active) < width and i < len(makers)
               and (not active or active[-1][1] >= min_gap)):
            gen = makers[i](); i += 1
            try:
                next(gen); active.append([gen, 0])
            except StopIteration:
                pass
        for ent in list(active):
            try:
                next(ent[0]); ent[1] += 1
            except StopIteration:
                active.remove(ent)


def build(n_tiles=8, layers=(0, 1), do_ffn=True, seq=SEQ):
    nc = bass.Bass("TRN2", target_bir_lowering=False)
    dr = {}

    def din(name, shape, dt=F32):
        dr[name] = nc.dram_tensor(name, list(shape), dt, kind="ExternalInput").ap()
        return dr[name]

    xT_d = din("xT", [128, 8, seq])
    wblk_d = din("wblk", [NBLK, 128, BLK])
    cst_d = din("cst", [128, NCST])
    nw_d = din("nw", [128, 5, 8]); wglow_d = din("wglow", [128, 8, 16]); wgu_d = din("wgu", [16, 512])
    bgate_d = din("bgate", [1, 512]); gnw_d = din("gnw", [128, 2]); wab_d = din("wab", [128, 8, 32])
    cw_d = din("cw", [128, 32, 4]); alog_d = din("alog", [1, 16]); dtb_d = din("dtb", [1, 16]); dnw_d = din("dnw", [128, 1])
    out_d = nc.dram_tensor("outT", [128, 8, seq], F32, kind="ExternalOutput").ap()
    wscr = nc.dram_tensor("wscr", [NBLK, 128, BLK], BF16, kind="Internal").ap()

    with ExitStack() as es:
        P = Prog(nc, es)
        hT = P.sbuf("hT", [128, 8, T], F32)
        hnT = P.sbuf("hnT", [128, 8, T], BF16)
        NSLOT = 4
        ring = [P.sbuf("ring%d" % i, [128, BLK], BF16) for i in range(NSLOT)]
        cst = P.sbuf("cst", [128, 896], F32)
        cstb = P.sbuf("cstb", [128, NCST], BF16)
        nw = P.sbuf("nw", [128, 5, 8], F32)
        wglow = P.sbuf("wglow", [128, 8, 16], BF16)
        wgu = P.sbuf("wgu", [16, 512], F32)
        bgate = P.sbuf("bgate", [1, 512], F32)
        gnw = P.sbuf("gnw", [128, 2], F32)
        wab = P.sbuf("wab", [128, 8, 32], BF16)
        cw = P.sbuf("cw", [128, 32, 4], F32)
        negA = P.sbuf("negA", [128, 16], F32)
        dtb = P.sbuf("dtb", [128, 16], F32)
        dnw = P.sbuf("dnw", [128, 1], F32)
        S1 = P.sbuf("S1", [128, 4, 256], F32); S1b = P.sbuf("S1b", [128, 4, 256], BF16)
        S2 = P.sbuf("S2", [128, 16, 128], F32); S2b = P.sbuf("S2b", [128, 16, 128], BF16)
        carry = P.sbuf("carry", [128, 32, 3], F32)
        banks = [P.psum("pb%d" % i, [128, 512], F32) for i in range(7)]
        psb = P.psum("psb", [128, 1024], BF16)
        bstate = [0]

        held = set()

        def bank(hold=False):
            for _ in range(8):
                b = banks[bstate[0] % 7]; bstate[0] += 1
                if b.name not in held:
                    if hold:
                        held.add(b.name)
                    return b
            raise RuntimeError("no free PSUM bank")

        def release(b):
            held.discard(b.name)

        ident = cst[:, C_IDENT:C_IDENT + 128]
        maskT = cst[:, C_MASKT:C_MASKT + 128]
        triS = cst[:, C_TRIS:C_TRIS + 128]
        Lstrict = cst[:, C_LSTRICT:C_LSTRICT + 128]
        maskD = cst[:, C_MASKD:C_MASKD + 128]
        maskOff = cst[:, C_MASKOFF:C_MASKOFF + 128]
        onesf = cst[:, C_ONES:C_ONES + 128]
        identb = cstb[:, C_IDENT:C_IDENT + 128]
        maskTb = cstb[:, C_MASKT:C_MASKT + 128]
        onesb = cstb[:, C_ONES:C_ONES + 128]

        P.dma('sp', cst, cst[:], None, cst_d[:, 0:896])
        P.dma('pool', cstb, cstb[:], None, cst_d)
        P.dma('sp', nw, nw[:], None, nw_d)
        P.dma('pool', wglow, wglow[:], None, wglow_d)
        P.dma('sp', wgu, wgu[:], None, wgu_d)
        P.dma('sp', bgate, bgate[:], None, bgate_d)
        P.dma('sp', gnw, gnw[:], None, gnw_d)
        P.dma('pool', wab, wab[:], None, wab_d)
        P.dma('sp', cw, cw[:], None, cw_d)
        P.dma('sp', negA, negA[:], None, alog_d.partition_broadcast(128))
        P.dma('sp', dtb, dtb[:], None, dtb_d.partition_broadcast(128))
        P.dma('sp', dnw, dnw[:], None, dnw_d)
        P.op('act', [negA], [negA], lambda g: g.activation(out=negA[:], in_=negA[:], func=AF.Exp))
        P.op('dve', [negA], [negA], lambda g: g.tensor_scalar(negA[:], negA[:], -1.0, None, op0=ALU.mult))
        for b_ in (S1, S1b, S2, S2b, carry):
            P.op('dve', [], [b_], lambda g, b_=b_: g.memset(b_[:], 0.0))

        need = []
        if 0 in layers:
            need += [n for n in BLOCKS if n.startswith("gla_")]
            if do_ffn:
                need += [n for n in BLOCKS if n.startswith("f0_")]
        if 1 in layers:
            need += [n for n in BLOCKS if n.startswith("gdn_")]
            if do_ffn:
                need += [n for n in BLOCKS if n.startswith("f1_")]
        NCAST = 8
        castb = [Buf("cast%d" % i, None) for i in range(NCAST)]
        cast_tok = {}
        for n_i, name in enumerate(need):
            bi = BIDX[name]
            cast_tok[name] = P.dma('pool', castb[n_i % NCAST], wscr[bi], None, wblk_d[bi])

        stream = [(t_, name) for t_ in range(n_tiles) for name in need]
        sstate = {"issued": 0, "used": 0}

        def issue_upto(k):
            while sstate["issued"] < min(k, len(stream)):
                i = sstate["issued"]
                t_, name = stream[i]
                slot = ring[i % NSLOT]
                if t_ == 0:
                    P.wait_tok('sp', cast_tok[name])
                P.dma('sp', slot, slot[:], None, wscr[BIDX[name]])
                sstate["issued"] += 1

        def wnext(expect):
            i = sstate["used"]
            assert stream[i][1] == expect, (stream[i], expect)
            issue_upto(i + NSLOT)
            sstate["used"] += 1
            return ring[i % NSLOT]

        nsq = [P.sbuf("nsq%d" % i, [128, T], BF16) for i in range(2)]
        nrstd = P.sbuf("nrstd", [128, T], F32)

        def rmsnorm(widx, out_final=None):
            bk = bank()
            for dc in range(8):
                q_ = nsq[dc % 2]
                P.op('act', [(hT, dc)], [q_], lambda g: g.activation(out=q_[:], in_=hT[:, dc, :], func=AF.Square))
                P.mm(bk, bk[:], cstb, onesb, q_, q_[:], start=(dc == 0), stop=(dc == 7))
            P.op('act', [bk], [nrstd], lambda g: g.activation(out=nrstd[:], in_=bk[:], func=AF.Ln, bias=NORM_EPS, scale=1.0 / D))
            P.op('act', [nrstd], [nrstd], lambda g: g.activation(out=nrstd[:], in_=nrstd[:], func=AF.Exp, scale=-0.5))
            dst = hnT if out_final is None else out_final
            for dc in range(8):
                P.op('dve', [(hT, dc), nw, nrstd], [(dst, dc)], lambda g: g.scalar_tensor_tensor(
                    dst[:, dc, :], hT[:, dc, :], nw[:, widx, dc:dc + 1], nrstd[:], op0=ALU.mult, op1=ALU.mult))

        def ffn(l):
            with ExitStack() as ls:
                actT = P.sbuf("actT", [128, NFC, T], BF16, ls)
                sg = [P.sbuf("sg%d" % i, [128, T], F32, ls) for i in range(2)]
                for b in range(11):
                    slot = wnext("f%d_gu%d" % (l, b))
                    sv = slot[:].rearrange("p (a b k c) -> p a b k c", a=2, b=2, k=8, c=128)
                    for f2 in range(2):
                        f = 2 * b + f2
                        bg = bank(); bu = bank()
                        for kc in range(8):
                            P.mm(bg, bg[:], slot, sv[:, f2, 0, kc, :], hnT, hnT[:, kc, :], start=(kc == 0), stop=(kc == 7))
                        for kc in range(8):
                            P.mm(bu, bu[:], slot, sv[:, f2, 1, kc, :], hnT, hnT[:, kc, :], start=(kc == 0), stop=(kc == 7))
                        s_ = sg[f % 2]
                        P.op('act', [bg], [s_], lambda g: g.activation(out=s_[:], in_=bg[:], func=AF.Silu))
                        P.op('dve', [s_, bu], [(actT, f)], lambda g: g.tensor_tensor(actT[:, f, :], s_[:], bu[:], op=ALU.mult))
                for dc in range(8):
                    slot = wnext("f%d_d%d" % (l, dc))
                    sv = slot[:, 0:DFF].rearrange("p (f c) -> p f c", c=128)
                    bk = bank()
                    for fc in range(NFC):
                        P.mm(bk, bk[:], slot, sv[:, fc, :], (actT, fc), actT[:, fc, :], start=(fc == 0), stop=(fc == NFC - 1))
                    P.op('dve', [(hT, dc), bk], [(hT, dc)], lambda g: g.tensor_tensor(hT[:, dc, :], hT[:, dc, :], bk[:], op=ALU.add))
                P.barrier()

        def gla():
            with ExitStack() as ls:
                glowT = P.sbuf("glowT", [16, T], F32, ls)
                tmpe = [P.sbuf("tmpe%d" % i, [128, 512], F32, ls) for i in range(4)]
                spb = [P.sbuf("spb%d" % i, [128, 512], F32, ls) for i in range(4)]
                EG = P.sbuf("EG", [128, 4, T], F32, ls)
                ENG = P.sbuf("ENG", [128, 4, T], F32, ls)
                qt = P.sbuf("qt", [128, 4, T], BF16, ls)
                kt = P.sbuf("kt", [128, 4, T], BF16, ls)
                vb = P.sbuf("vb", [128, NCH, 1024], BF16, ls)
                sr = P.sbuf("sr", [128, 8, T], BF16, ls)
                scm_all = P.sbuf("scm", [128, NCH, 4, 128], BF16, ls)
                ktok_all = P.sbuf("ktok", [128, NCH, 4, 128], BF16, ls)
                sqo_l = [P.sbuf("sqo%d" % i, [128, 8, 128], BF16, ls) for i in range(2)]
                std_l = [None, None]
                rstd_l = [P.sbuf("grstd%d" % i, [128, 4, 128], F32, ls) for i in range(2)]
                t1_l = [P.sbuf("gt1%d" % i, [128, 8, 128], F32, ls) for i in range(2)]
                bk = bank()
                for kc in range(8):
                    P.mm(bk, bk[0:16, :], wglow, wglow[:, kc, :], hnT, hnT[:, kc, :], start=(kc == 0), stop=(kc == 7))
                P.op('act', [bk], [glowT], lambda g: g.activation(out=glowT[:], in_=bk[0:16, :], func=AF.Copy))
                def chainG(c):
                    cs = slice(c * 128, (c + 1) * 128)
                    te = tmpe[c % 4]; sp_ = spb[c % 4]
                    bx = bank()
                    P.mm(bx, bx[:], glowT, glowT[:, cs], wgu, wgu[:], start=True, stop=False)
                    P.mm(bx, bx[:], cst, onesf[0:1, :], bgate, bgate[:], start=False, stop=True)
                    P.op('act', [bx], [te], lambda g: g.activation(out=te[:], in_=bx[:], func=AF.Exp, scale=-1.0))
                    yield
                    P.op('act', [te], [sp_], lambda g: g.activation(out=sp_[:], in_=te[:], func=AF.Ln, bias=1.0))
                    yield
                    bG = bank()
                    for h in range(4):
                        P.mm(bG, bG[:, h * 128:(h + 1) * 128], sp_, sp_[:, h * 128:(h + 1) * 128], cst, triS)
                    bGv = bG[:].rearrange("p (h c) -> p h c", h=4)
                    P.op('act', [bG], [(EG, c)], lambda g: g.activation(out=EG[:, :, cs], in_=bGv, func=AF.Exp))
                    P.op('act', [bG], [(ENG, c)], lambda g: g.activation(out=ENG[:, :, cs], in_=bGv, func=AF.Exp, scale=-1.0))
                run_pipeline([(lambda c=c: chainG(c)) for c in range(NCH)], 4, min_gap=0)
                slot = wnext("gla_q"); sv = slot[:].rearrange("p (h k c) -> p h k c", h=4, k=8, c=128)
                for h in range(4):
                    bk = bank()
                    for kc in range(8):
                        P.mm(bk, bk[:], slot, sv[:, h, kc, :], hnT, hnT[:, kc, :], start=(kc == 0), stop=(kc == 7))
                    P.op('dve', [bk, EG], [(qt, h)], lambda g: g.scalar_tensor_tensor(
                        qt[:, h, :], bk[:], 128.0 ** -0.5, EG[:, h, :], op0=ALU.mult, op1=ALU.mult))
                slot = wnext("gla_k"); sv = slot[:].rearrange("p (h k c) -> p h k c", h=4, k=8, c=128)
                for h in range(4):
                    bk = bank()
                    for kc in range(8):
                        P.mm(bk, bk[:], slot, sv[:, h, kc, :], hnT, hnT[:, kc, :], start=(kc == 0), stop=(kc == 7))
                    P.op('dve', [bk, ENG], [(kt, h)], lambda g: g.tensor_tensor(kt[:, h, :], bk[:], ENG[:, h, :], op=ALU.mult))
                for cg in range(2):
                    slot = wnext("gla_v%d" % cg); sv = slot[:].rearrange("p (k c) -> p k c", k=8, c=512)
                    for c in range(NCH):
                        bk = bank()
                        for kc in range(8):
                            P.mm(bk, bk[:], hnT, hnT[:, kc, c * 128:(c + 1) * 128], slot, sv[:, kc, :], start=(kc == 0), stop=(kc == 7))
                        P.op('act', [bk], [(vb, (c, cg))], lambda g: g.activation(out=vb[:, c, cg * 512:(cg + 1) * 512], in_=bk[:], func=AF.Copy))
                for cg in range(2):
                    slot = wnext("gla_r%d" % cg); sv = slot[:].rearrange("p (e k c) -> p e k c", e=4, k=8, c=128)
                    for e4 in range(4):
                        bk = bank()
                        for kc in range(8):
                            P.mm(bk, bk[:], slot, sv[:, e4, kc, :], hnT, hnT[:, kc, :], start=(kc == 0), stop=(kc == 7))
                        P.op('act', [bk], [(sr, cg * 4 + e4)], lambda g: g.activation(out=sr[:, cg * 4 + e4, :], in_=bk[:], func=AF.Silu))
                for c in range(NCH):
                    cs = slice(c * 128, (c + 1) * 128)
                    bs = bank()
                    for h in range(4):
                        P.mm(bs, bs[:, h * 128:(h + 1) * 128], (kt, h), kt[:, h, cs], (qt, h), qt[:, h, cs])
                    P.op('dve', [bs, cst], [(scm_all, c)], lambda g: g.tensor_tensor(
                        scm_all[:, c], bs[:].rearrange("p (h c) -> p h c", h=4),
                        maskT.unsqueeze(1).broadcast_to([128, 4, 128]), op=ALU.mult))
                    for h in range(4):
                        P.op('pe', [kt, cstb], [psb], lambda g: g.transpose(psb[:, h * 128:(h + 1) * 128], kt[:, h, cs], identb))
                    P.op('act', [psb], [(ktok_all, c)], lambda g: g.activation(
                        out=ktok_all[:, c], in_=psb[:, 0:512].rearrange("p (h c) -> p h c", h=4), func=AF.Copy))
                def chainC(c):
                    cs = slice(c * 128, (c + 1) * 128)
                    scm = scm_all[:, c]; ktok = ktok_all[:, c]
                    sqo = sqo_l[c % 2]; std = std_l[c % 2]; rstd = rstd_l[c % 2]; t1 = t1_l[c % 2]
                    bo = [bank(hold=True), bank(hold=True)]
                    for h in range(4):
                        for eh in range(2):
                            o_ap = bo[h // 2][:, ((h % 2) * 2 + eh) * 128:((h % 2) * 2 + eh + 1) * 128]
                            P.mm(bo[h // 2], o_ap, vb, vb[:, c, h * 256 + eh * 128:h * 256 + (eh + 1) * 128], (scm_all, c), scm[:, h, :],
                                 start=True, stop=False)
                            P.mm(bo[h // 2], o_ap, S1b, S1b[:, h, eh * 128:(eh + 1) * 128], (qt, h), qt[:, h, cs],
                                 start=False, stop=True)
                    bS = [bank(), bank()]
                    for h in range(4):
                        P.mm(bS[h // 2], bS[h // 2][:, (h % 2) * 256:(h % 2 + 1) * 256], (ktok_all, c), ktok[:, h, :],
                             vb, vb[:, c, h * 256:(h + 1) * 256])
                    for b2 in range(2):
                        P.op('dve', [(S1, b2), bS[b2]], [(S1, b2)], lambda g: g.tensor_tensor(
                            S1[:, 2 * b2:2 * b2 + 2, :], S1[:, 2 * b2:2 * b2 + 2, :],
                            bS[b2][:].rearrange("p (h c) -> p h c", h=2), op=ALU.add))
                    last = c * 128 + 127
                    P.op('dve', [S1, EG], [S1], lambda g: g.tensor_tensor(
                        S1[:], S1[:], EG[:, :, last:last + 1].broadcast_to([128, 4, 256]), op=ALU.mult))
                    P.op('act', [S1], [S1b], lambda g: g.activation(out=S1b[:], in_=S1[:], func=AF.Copy))
                    yield
                    for b2 in range(2):
                        P.op('act', [bo[b2]], [(sqo, b2)], lambda g: g.activation(
                            out=sqo[:, 4 * b2:4 * b2 + 4, :], in_=bo[b2][:].rearrange("p (h c) -> p h c", h=4), func=AF.Square))
                    bq = bank()
                    for h in range(4):
                        for eh in range(2):
                            P.mm(bq, bq[:, h * 128:(h + 1) * 128], cstb, onesb, sqo, sqo[:, 2 * h + eh, :],
                                 start=(eh == 0), stop=(eh == 1))
                    P.op('act', [bq], [rstd], lambda g: g.activation(
                        out=rstd[:], in_=bq[:].rearrange("p (h c) -> p h c", h=4), func=AF.Ln, bias=NORM_EPS, scale=1.0 / 256))
                    P.op('act', [rstd], [rstd], lambda g: g.activation(out=rstd[:], in_=rstd[:], func=AF.Exp, scale=-0.5))
                    yield
                    for b2 in range(2):
                        P.op('dve', [bo[b2], rstd], [(t1, b2)], lambda g: g.tensor_tensor(
                            t1[:, 4 * b2:4 * b2 + 4, :].rearrange("p (h e) c -> p h e c", h=2),
                            bo[b2][:].rearrange("p (h e c) -> p h e c", h=2, e=2),
                            rstd[:, 2 * b2:2 * b2 + 2, :].unsqueeze(2).broadcast_to([128, 2, 2, 128]), op=ALU.mult))
                        release(bo[b2])
                    for eh in range(2):
                        t1v = t1[:].rearrange("p (h e) c -> p h e c", e=2)[:, :, eh, :]
                        srv = sr[:, :, cs].rearrange("p (h e) c -> p h e c", e=2)[:, :, eh, :]
                        P.op('dve', [t1, gnw, sr], [sr], lambda g: g.scalar_tensor_tensor(
                            srv, t1v, gnw[:, eh:eh + 1], srv, op0=ALU.mult, op1=ALU.mult))
                run_pipeline([(lambda c=c: chainC(c)) for c in range(NCH)], 2, min_gap=1)
                for cg in range(2):
                    slot = wnext("gla_o%d" % cg); sv = slot[:].rearrange("p (d k c) -> p d k c", d=4, k=8, c=128)
                    for d4 in range(4):
                        dc = cg * 4 + d4
                        bk = bank()
                        for ec in range(8):
                            P.mm(bk, bk[:], slot, sv[:, d4, ec, :], (sr, ec), sr[:, ec, :], start=(ec == 0), stop=(ec == 7))
                        P.op('dve', [(hT, dc), bk], [(hT, dc)], lambda g: g.tensor_tensor(hT[:, dc, :], hT[:, dc, :], bk[:], op=ALU.add))
                P.barrier()

        def gdn():
            with ExitStack() as ls:
                qn = P.sbuf("qn", [128, 8, T], BF16, ls)
                kn = P.sbuf("kn", [128, 8, T], BF16, ls)
                vtok = P.sbuf("vtok", [128, NCH, 2048], BF16, ls)
                zs = P.sbuf("zs", [128, 16, T], BF16, ls)
                with ExitStack() as la:
                    NA = 5
                    A_ = []
                    for i in range(NA):
                        A_.append(dict(
                            xp=P.sbuf("xpre%d" % i, [128, 3 + T], F32, la), ac=P.sbuf("cacc%d" % i, [128, T], F32, la),
                            sqy=P.sbuf("sqy%d" % i, [128, T], BF16, la), vTc=P.sbuf("vTc%d" % i, [128, T], BF16, la)))
                    ssq_all = P.sbuf("ssq_all", [128, 16, T], F32, la)

                    def chainA(cc, slot, sv, c4, B):
                        xp = B["xp"]; ac = B["ac"]; sqy = B["sqy"]; vTc = B["vTc"]
                        bk = bank()
                        for kc in range(8):
                            P.mm(bk, bk[:], slot, sv[:, c4, kc, :], hnT, hnT[:, kc, :], start=(kc == 0), stop=(kc == 7))
                        CE = 'dve' if DBG.get('carrydve') else 'pool'
                        P.op(CE, [(carry, cc)], [xp], lambda g: g.tensor_copy(xp[:, 0:3], carry[:, cc, :]))
                        P.op('act', [bk], [xp], lambda g: g.activation(out=xp[:, 3:3 + T], in_=bk[:], func=AF.Copy))
                        yield
                        P.op(CE, [xp], [(carry, cc)], lambda g: g.tensor_copy(carry[:, cc, :], xp[:, T:T + 3]))
                        P.op('dve' if DBG.get('tapdve') else 'pool', [xp, cw], [ac], lambda g: g.tensor_scalar(
                            ac[:], xp[:, 3:3 + T], cw[:, cc, 3:4], None, op0=ALU.mult))
                        yield
                        for k in (2, 1, 0):
                            P.op('dve', [xp, cw, ac], [ac], lambda g: g.scalar_tensor_tensor(
                                ac[:], xp[:, k:k + T], cw[:, cc, k:k + 1], ac[:], op0=ALU.mult, op1=ALU.add))
                        yield
                        if cc < 16:
                            dst = qn if cc < 8 else kn
                            P.op('act', [ac], [(dst, cc % 8)], lambda g: g.activation(out=dst[:, cc % 8, :], in_=ac[:], func=AF.Silu))
                            P.op('act', [(dst, cc % 8)], [sqy], lambda g: g.activation(out=sqy[:], in_=dst[:, cc % 8, :], func=AF.Square))
                            yield
                            bq = bank()
                            P.mm(bq, bq[:], cstb, onesb, sqy, sqy[:])
                            P.op('act', [bq], [(ssq_all, cc)], lambda g: g.activation(out=ssq_all[:, cc, :], in_=bq[:], func=AF.Copy))
                        else:
                            P.op('act', [ac], [vTc], lambda g: g.activation(out=vTc[:], in_=ac[:], func=AF.Silu))
                            yield
                            for c in range(NCH):
                                P.op('pe', [vTc, cstb], [psb], lambda g: g.transpose(
                                    psb[:, c * 128:(c + 1) * 128], vTc[:, c * 128:(c + 1) * 128], identb))
                            vc = cc - 16
                            P.op('dve', [psb], [(vtok, vc)], lambda g: g.tensor_copy(
                                vtok[:, :, vc * 128:(vc + 1) * 128], psb[:, 0:512].rearrange("p (c e) -> p c e", c=NCH)))

                    cur = {}

                    def mkA(cc):
                        def f():
                            if cc % 4 == 0:
                                cur["slot"] = wnext("gdn_qkv%d" % (cc // 4))
                                cur["sv"] = cur["slot"][:].rearrange("p (a k c) -> p a k c", a=4, k=8, c=128)
                            return chainA(cc, cur["slot"], cur["sv"], cc % 4, A_[cc % NA])
                        return f
                    run_pipeline([mkA(cc) for cc in range(32)], NA, min_gap=1)
                    for b in range(4):
                        slot = wnext("gdn_z%d" % b); sv = slot[:].rearrange("p (a k c) -> p a k c", a=4, k=8, c=128)
                        for c4 in range(4):
                            bk = bank()
                            for kc in range(8):
                                P.mm(bk, bk[:], slot, sv[:, c4, kc, :], hnT, hnT[:, kc, :], start=(kc == 0), stop=(kc == 7))
                            P.op('act', [bk], [(zs, 4 * b + c4)], lambda g: g.activation(out=zs[:, 4 * b + c4, :], in_=bk[:], func=AF.Silu))
                    import math
                    for qi, dst in enumerate((qn, kn)):
                        sl = ssq_all[:, 8 * qi:8 * qi + 8, :]
                        P.op('act', [ssq_all], [(ssq_all, ('h', qi))], lambda g: g.activation(out=sl, in_=sl, func=AF.Ln, bias=L2_EPS, scale=1.0))
                        P.op('act', [ssq_all], [(ssq_all, ('h', qi))], lambda g: g.activation(
                            out=sl, in_=sl, func=AF.Exp, scale=-0.5, bias=(-0.5 * math.log(128.0) if qi == 0 else 0.0)))
                        P.op('dve', [ssq_all, dst], [dst], lambda g: g.tensor_tensor(dst[:], dst[:], sl, op=ALU.mult))
                    P.barrier()
                with ExitStack() as lb:
                    def sb(name, shape, dt):
                        return P.sbuf(name, list(shape), dt, lb)
                    pc = []
                    for par in range(2):
                        d_ = {}
                        for nm in ("beta", "lnbeta", "tz", "gg", "Gcol", "negEG", "dcol", "bd", "EGL"):
                            d_[nm] = sb(nm + str(par), (128, 16), F32)
                        d_["eGT"] = sb("eGT" + str(par), (16, 128), BF16)
                        d_["ktk"] = sb("ktk" + str(par), (128, 8, 128), BF16)
                        pc.append(d_)
                    sets = []
                    for si in range(NSETS):
                        d_ = {}
                        for nm in ("KKD", "KKO", "QKm"):
                            d_[nm] = sb(nm + str(si), (128, 2, 128), F32)
                        for nm in ("lhs4", "bdec"):
                            d_[nm] = sb(nm + str(si), (128, 4, 128), F32)
                        for nm in ("NTa", "NTb", "Na", "Nb", "TTa", "TTb", "MTo", "x0", "attnT", "qg", "vdec"):
                            d_[nm] = sb(nm + str(si), (128, 4, 128), BF16)
                        d_["x1"] = d_["NTa"]; d_["x2"] = d_["NTb"]; d_["sqo"] = d_["Na"]; d_["vpre"] = d_["Nb"]
                        sets.append(d_)
                    v4 = lambda ap: ap.rearrange("p (h c) -> p h c", h=4)
                    selb = cstb[0:16, C_SELB:C_SELB + 2048].rearrange("p (h c) -> p h c", h=16)

                    def preamble(c):
                        q_ = pc[c % 2]
                        beta = q_["beta"]; lnbeta = q_["lnbeta"]; tz = q_["tz"]; gg = q_["gg"]; Gcol = q_["Gcol"]
                        negEG = q_["negEG"]; dcol = q_["dcol"]; bd = q_["bd"]; EGL = q_["EGL"]; eGT = q_["eGT"]; ktk = q_["ktk"]
                        cs = slice(c * 128, (c + 1) * 128)
                        bab = bank()
                        for kc in range(8):
                            P.mm(bab, bab[:, 0:32], hnT, hnT[:, kc, cs], wab, wab[:, kc, :], start=(kc == 0), stop=(kc == 7))
                        P.op('act', [bab], [lnbeta], lambda g: g.activation(out=lnbeta[:], in_=bab[:, 0:16], func=AF.Exp, scale=-1.0))
                        P.op('dve', [bab, dtb], [tz], lambda g: g.tensor_tensor(tz[:], bab[:, 16:32], dtb[:], op=ALU.add))
                        P.op('act', [lnbeta], [lnbeta], lambda g: g.activation(out=lnbeta[:], in_=lnbeta[:], func=AF.Ln, bias=1.0))
                        P.op('pool', [lnbeta], [lnbeta], lambda g: g.tensor_scalar(lnbeta[:], lnbeta[:], -1.0, None, op0=ALU.mult))
                        P.op('act', [lnbeta], [beta], lambda g: g.activation(out=beta[:], in_=lnbeta[:], func=AF.Exp))
                        P.op('act', [tz], [tz], lambda g: g.activation(out=tz[:], in_=tz[:], func=AF.Exp))
                        P.op('act', [tz], [tz], lambda g: g.activation(out=tz[:], in_=tz[:], func=AF.Ln, bias=1.0))
                        P.op('dve', [tz, negA], [gg], lambda g: g.tensor_tensor(gg[:], tz[:], negA[:], op=ALU.mult))
                        bG = bank()
                        P.mm(bG, bG[:, 0:16], cst, maskT, gg, gg[:])
                        P.mm(bG, bG[:, 16:32], cst, onesf, gg, gg[:])
                        P.mm(bG, bG[0:16, 128:256], gg, gg[:], cst, maskT)
                        P.op('act', [bG], [Gcol], lambda g: g.activation(out=Gcol[:], in_=bG[:, 0:16], func=AF.Copy))
                        P.op('act', [bG], [negEG], lambda g: g.activation(out=negEG[:], in_=bG[:, 0:16], func=AF.Exp))
                        P.op('act', [bG], [EGL], lambda g: g.activation(out=EGL[:], in_=bG[:, 16:32], func=AF.Exp))
                        P.op('act', [bG], [eGT], lambda g: g.activation(out=eGT[:], in_=bG[0:16, 128:256], func=AF.Exp))
                        P.op('dve', [bG, Gcol], [dcol], lambda g: g.tensor_tensor(dcol[:], bG[:, 16:32], Gcol[:], op=ALU.subtract))
                        P.op('pool', [negEG], [negEG], lambda g: g.tensor_scalar(negEG[:], negEG[:], -1.0, None, op0=ALU.mult))
                        P.op('act', [dcol], [dcol], lambda g: g.activation(out=dcol[:], in_=dcol[:], func=AF.Exp))
                        P.op('pool', [dcol, beta], [bd], lambda g: g.tensor_tensor(bd[:], dcol[:], beta[:], op=ALU.mult))
                        for gq in range(8):
                            P.op('pe', [(kn, gq), cstb], [psb], lambda g: g.transpose(psb[:, gq * 128:(gq + 1) * 128], kn[:, gq, cs], identb))
                        P.op('act', [psb], [ktk], lambda g: g.activation(
                            out=ktk[:], in_=psb[:].rearrange("p (h c) -> p h c", h=8), func=AF.Copy))

                    def chain(c, grp, s_):
                        q_ = pc[c % 2]
                        lnbeta = q_["lnbeta"]; gg = q_["gg"]
                        negEG = q_["negEG"]; bd = q_["bd"]; EGL = q_["EGL"]; eGT = q_["eGT"]; ktk = q_["ktk"]
                        KKD = s_["KKD"]; KKO = s_["KKO"]; QKm = s_["QKm"]; lhs4 = s_["lhs4"]; bdec = s_["bdec"]
                        MTo = s_["MTo"]; x0 = s_["x0"]; x1 = s_["x1"]; x2 = s_["x2"]
                        attnT = s_["attnT"]; qg = s_["qg"]; vpre = s_["vpre"]; vdec = s_["vdec"]; sqo = s_["sqo"]
                        rstd = s_["lhs4"]
                        NTl = [s_["NTa"], s_["NTb"]]; Nl = [s_["Na"], s_["Nb"]]; TTl = [s_["TTa"], s_["TTb"]]
                        cs = slice(c * 128, (c + 1) * 128)
                        h0 = 4 * grp; g0 = 2 * grp
                        P.op('pool', [cst, gg], [lhs4], lambda g: g.tensor_tensor(
                            lhs4[:], Lstrict.unsqueeze(1).broadcast_to([128, 4, 128]),
                            gg[:, h0:h0 + 4].unsqueeze(2).broadcast_to([128, 4, 128]), op=ALU.mult))
                        bkk = bank()
                        for gi in range(2):
                            P.mm(bkk, bkk[:, gi * 128:(gi + 1) * 128], (kn, g0 + gi), kn[:, g0 + gi, cs], (kn, g0 + gi), kn[:, g0 + gi, cs])
                            P.mm(bkk, bkk[:, (2 + gi) * 128:(3 + gi) * 128], (kn, g0 + gi), kn[:, g0 + gi, cs], (qn, g0 + gi), qn[:, g0 + gi, cs])
                        kk2 = bkk[:, 0:256].rearrange("p (h c) -> p h c", h=2)
                        P.op('dve', [bkk, cst], [KKD], lambda g: g.tensor_tensor(
                            KKD[:], kk2, maskD.unsqueeze(1).broadcast_to([128, 2, 128]), op=ALU.mult))
                        P.op('dve', [bkk, cst], [KKO], lambda g: g.tensor_tensor(
                            KKO[:], kk2, maskOff.unsqueeze(1).broadcast_to([128, 2, 128]), op=ALU.mult))
                        P.op('dve', [bkk, cst], [QKm], lambda g: g.tensor_tensor(
                            QKm[:], bkk[:, 256:512].rearrange("p (h c) -> p h c", h=2),
                            maskT.unsqueeze(1).broadcast_to([128, 2, 128]), op=ALU.mult))
                        yield
                        bd_ = bank()
                        for hh in range(4):
                            P.mm(bd_, bd_[:, hh * 128:(hh + 1) * 128], lhs4, lhs4[:, hh, :], cst, maskT)
                        for hh in range(4):
                            P.op('act', [bd_, lnbeta], [(bdec, hh)], lambda g: g.activation(
                                out=bdec[:, hh, :], in_=bd_[:, hh * 128:(hh + 1) * 128], func=AF.Exp,
                                bias=lnbeta[:, h0 + hh:h0 + hh + 1], scale=1.0))
                        bks = bank()
                        for hh in range(4):
                            P.mm(bks, bks[:, hh * 128:(hh + 1) * 128], (kn, (h0 + hh) // 2), kn[:, (h0 + hh) // 2, cs], (S2b, grp), S2b[:, h0 + hh, :])
                        for hh in range(4):
                            h = h0 + hh
                            P.op('dve', [bks, negEG, (vtok, h)], [(x0, hh)], lambda g: g.scalar_tensor_tensor(
                                x0[:, hh, :], bks[:, hh * 128:(hh + 1) * 128], negEG[:, h:h + 1],
                                vtok[:, c, h * 128:(h + 1) * 128], op0=ALU.mult, op1=ALU.add))
                        bq = bank()
                        for hh in range(4):
                            P.mm(bq, bq[:, hh * 128:(hh + 1) * 128], cstb, selb[:, h0 + hh, :], eGT, eGT[:])
                        P.op('dve', [bq, qn], [qg], lambda g: g.tensor_tensor(
                            qg[:].rearrange("p (a b) c -> p a b c", a=2),
                            bq[:].rearrange("p (a b c) -> p a b c", a=2, b=2),
                            qn[:, g0:g0 + 2, cs].unsqueeze(2).broadcast_to([128, 2, 2, 128]), op=ALU.mult))
                        yield
                        NT = NTl[0]; N_ = Nl[0]
                        pair = lambda b_: b_[:].unsqueeze(2).broadcast_to([128, 2, 2, 128])
                        r4 = lambda b_: b_[:].rearrange("p (a b) c -> p a b c", a=2)
                        P.op('dve', [KKD, bdec], [NT], lambda g: g.tensor_tensor(r4(NT), r4(bdec), pair(KKD), op=ALU.mult))
                        for hh in range(4):
                            P.op('pe', [NT, cstb], [psb], lambda g: g.transpose(psb[:, hh * 128:(hh + 1) * 128], NT[:, hh, :], identb))
                        P.op('act', [psb], [N_], lambda g: g.activation(out=N_[:], in_=v4(psb[:, 0:512]), func=AF.Copy))
                        TT = TTl[0]
                        P.op('pool', [NT, cstb], [TT], lambda g: g.tensor_tensor(
                            TT[:], NT[:], identb.unsqueeze(1).broadcast_to([128, 4, 128]), op=ALU.add))
                        P.op('dve', [KKO, bdec], [MTo], lambda g: g.tensor_tensor(r4(MTo), r4(bdec), pair(KKO), op=ALU.mult))
                        P.op('dve', [QKm, bdec], [attnT], lambda g: g.tensor_tensor(r4(attnT), r4(bdec), pair(QKm), op=ALU.mult))
                        yield
                        for lvl in range(5):
                            NTc = NTl[lvl % 2]; Nc = Nl[lvl % 2]
                            NTn = NTl[(lvl + 1) % 2]; Nn = Nl[(lvl + 1) % 2]
                            TTo = TTl[lvl % 2]; TTn = TTl[(lvl + 1) % 2]
                            bN = bank()
                            for hh in range(4):
                                P.mm(bN, bN[:, hh * 128:(hh + 1) * 128], NTc, NTc[:, hh, :], Nc, Nc[:, hh, :])
                            if lvl < 4:
                                bNT = bank()
                                for hh in range(4):
                                    P.mm(bNT, bNT[:, hh * 128:(hh + 1) * 128], Nc, Nc[:, hh, :], NTc, NTc[:, hh, :])
                            P.op('act', [bN], [Nn], lambda g: g.activation(out=Nn[:], in_=v4(bN[:]), func=AF.Copy))
                            if lvl < 4:
                                P.op('dve', [bNT], [NTn], lambda g: g.tensor_copy(NTn[:], v4(bNT[:])))
                            yield
                            bT = bank()
                            for hh in range(4):
                                o_ap = bT[:, hh * 128:(hh + 1) * 128]
                                P.mm(bT, o_ap, Nn, Nn[:, hh, :], TTo, TTo[:, hh, :], start=True, stop=False)
                                P.mm(bT, o_ap, cstb, identb, TTo, TTo[:, hh, :], start=False, stop=True)
                            if lvl % 2 == 0:
                                P.op('act', [bT], [TTn], lambda g: g.activation(out=TTn[:], in_=v4(bT[:]), func=AF.Copy))
                            else:
                                P.op('dve', [bT], [TTn], lambda g: g.tensor_copy(TTn[:], v4(bT[:])))
                            yield
                        TT = TTl[5 % 2]
                        b1 = bank()
                        for hh in range(4):
                            P.mm(b1, b1[:, hh * 128:(hh + 1) * 128], TT, TT[:, hh, :], (x0, hh), x0[:, hh, :])
                        P.op('act', [b1], [x1], lambda g: g.activation(out=x1[:], in_=v4(b1[:]), func=AF.Copy))
                        yield
                        b2 = bank()
                        for hh in range(4):
                            P.mm(b2, b2[:, hh * 128:(hh + 1) * 128], MTo, MTo[:, hh, :], x1, x1[:, hh, :])
                        P.op('dve', [b2, x0], [x2], lambda g: g.tensor_tensor(x2[:], x0[:], v4(b2[:]), op=ALU.subtract))
                        yield
                        b3 = bank()
                        for hh in range(4):
                            P.mm(b3, b3[:, hh * 128:(hh + 1) * 128], TT, TT[:, hh, :], x2, x2[:, hh, :])
                        P.op('act', [b3], [vpre], lambda g: g.activation(out=vpre[:], in_=v4(b3[:]), func=AF.Copy))
                        P.op('dve', [b3, bd], [vdec], lambda g: g.tensor_tensor(
                            vdec[:], v4(b3[:]), bd[:, h0:h0 + 4].unsqueeze(2).broadcast_to([128, 4, 128]), op=ALU.mult))
                        yield
                        bo = bank(hold=True)
                        for hh in range(4):
                            o_ap = bo[:, hh * 128:(hh + 1) * 128]
                            P.mm(bo, o_ap, vpre, vpre[:, hh, :], attnT, attnT[:, hh, :], start=True, stop=False)
                            P.mm(bo, o_ap, (S2b, grp), S2b[:, h0 + hh, :], qg, qg[:, hh, :], start=False, stop=True)
                        bS = bank()
                        for hh in range(4):
                            P.mm(bS, bS[:, hh * 128:(hh + 1) * 128], ktk, ktk[:, (h0 + hh) // 2, :], vdec, vdec[:, hh, :])
                        P.op('pool', [(S2, grp), EGL], [(S2, grp)], lambda g: g.tensor_tensor(
                            S2[:, h0:h0 + 4, :], S2[:, h0:h0 + 4, :],
                            EGL[:, h0:h0 + 4].unsqueeze(2).broadcast_to([128, 4, 128]), op=ALU.mult))
                        P.op('act', [bo], [sqo], lambda g: g.activation(out=sqo[:], in_=v4(bo[:]), func=AF.Square))
                        P.op('dve', [(S2, grp), bS], [(S2, grp)], lambda g: g.tensor_tensor(
                            S2[:, h0:h0 + 4, :], S2[:, h0:h0 + 4, :], v4(bS[:]), op=ALU.add))
                        P.op('act', [(S2, grp)], [(S2b, grp)], lambda g: g.activation(out=S2b[:, h0:h0 + 4, :], in_=S2[:, h0:h0 + 4, :], func=AF.Copy))
                        yield
                        bn = bank()
                        P.mm(bn, bn[:], cstb, onesb, sqo, sqo[:].rearrange("p h c -> p (h c)"))
                        P.op('act', [bn], [rstd], lambda g: g.activation(out=rstd[:], in_=v4(bn[:]), func=AF.Ln, bias=NORM_EPS, scale=1.0 / 128))
                        P.op('act', [rstd], [rstd], lambda g: g.activation(out=rstd[:], in_=rstd[:], func=AF.Exp, scale=-0.5))
                        yield
                        P.op('dve', [rstd] + [(zs, h0 + i_) for i_ in range(4)], [(zs, h0 + i_) for i_ in range(4)], lambda g: g.tensor_tensor(
                            zs[:, h0:h0 + 4, cs], zs[:, h0:h0 + 4, cs], rstd[:], op=ALU.mult))
                        P.op('dve', [bo, dnw] + [(zs, h0 + i_) for i_ in range(4)], [(zs, h0 + i_) for i_ in range(4)], lambda g: g.scalar_tensor_tensor(
                            zs[:, h0:h0 + 4, cs], v4(bo[:]), dnw[:, 0:1], zs[:, h0:h0 + 4, cs], op0=ALU.mult, op1=ALU.mult))
                        release(bo)

                    def mkB(ti):
                        def f():
                            c_, grp_ = ti // 4, ti % 4
                            if grp_ == 0:
                                preamble(c_)
                            return chain(c_, grp_, sets[ti % NSETS])
                        return f
                    run_pipeline([mkB(ti) for ti in range(NCH * 4)], NSETS, min_gap=B_GAP)
                    P.barrier()
                for b in range(4):
                    slot = wnext("gdn_o%d" % b); sv = slot[:].rearrange("p (d k c) -> p d k c", d=2, k=16, c=128)
                    for d2 in range(2):
                        dc = 2 * b + d2
                        bk = bank()
                        for ec in range(16):
                            P.mm(bk, bk[:], slot, sv[:, d2, ec, :], (zs, ec), zs[:, ec, :], start=(ec == 0), stop=(ec == 15))
                        P.op('dve', [(hT, dc), bk], [(hT, dc)], lambda g: g.tensor_tensor(hT[:, dc, :], hT[:, dc, :], bk[:], op=ALU.add))
                P.barrier()

        for t_ in range(n_tiles):
            ts = slice(t_ * T, (t_ + 1) * T)
            for dc_ in range(8):
                P.dma('sp', (hT, dc_), hT[:, dc_, :], None, xT_d[:, dc_, ts], semkey='hT%d' % dc_)
            issue_upto(NSLOT)
            if 0 in layers:
                rmsnorm(0); gla()
                if do_ffn:
                    rmsnorm(1); ffn(0)
            if 1 in layers:
                rmsnorm(2); gdn()
                if do_ffn:
                    rmsnorm(3); ffn(1)
            with ExitStack() as lo:
                oT = P.sbuf("oT", [128, 8, T], F32, lo)
                rmsnorm(4, out_final=oT)
                for dc_ in range(8):
                    last_tok = P.dma('sp', None, out_d[:, dc_, ts], (oT, dc_), oT[:, dc_, :], semkey="outst")
                for e_ in ('pe', 'act', 'dve', 'pool'):
                    P.wait_tok(e_, last_tok)
        P.wait_tok('sp', last_tok)
        P.barrier()
        build.stats = (P.ninstr, P.nwaits, dict(P.cnt))
    return nc


def prep_inputs(inp, n_cores=8, seq=SEQ):
    wb = make_wblocks(inp)
    cst = make_consts()
    sm = make_small(inp)
    maps = []
    for b in range(n_cores):
        xT = np.ascontiguousarray(inp["x"][b, :seq].T.reshape(8, 128, seq).transpose(1, 0, 2))
        m = {"xT": xT, "wblk": wb, "cst": cst}
        m.update(sm)
        maps.append(m)
    return maps


def kernel(**inputs):
    inp = {k: np.asarray(v, dtype=np.float32) for k, v in inputs.items()}
    nc = build()
    maps = prep_inputs(inp)
    res = run_bass_kernel_spmd(nc, maps, core_ids=list(range(8)))
    outs = []
    for b in range(8):
        oT = res.results[b]["outT"]
        outs.append(oT.transpose(2, 1, 0).reshape(SEQ, D))
    return np.stack(outs, axis=0).astype(np.float32)
```
